# Optimizing a Trainium2 kernel written in Bass

```python
import math
import jax, jax.numpy as jnp
from jax import lax
import numpy as np

D_MODEL = 1024
BATCH = 2
SEQ = 16384
DEPTH = 2

CHUNK = 64
DN_HEADS = 8
DN_DK = 128
DN_DV = 128
DN_WIDTH = DN_HEADS * DN_DK
CONV_K = 4
SA_HEADS = 8
SA_HD = 128
SA_WIDTH = SA_HEADS * SA_HD
IDX_HEADS = 8
IDX_HD = 64
TOPK_MAX = 256
Q_BLOCK = 128
XA_HEADS = 4
XA_HD = 256
XA_WIDTH = XA_HEADS * XA_HD
N_MEM = 256
ROPE_THETA = 500000.0
ROPE_DIV = 4
D_FF = ((8 * D_MODEL // 3 + 255) // 256) * 256
N_BRANCH = 3
NORM_EPS = 1e-6

IN_SIZES = (DN_WIDTH, DN_WIDTH, DN_HEADS * DN_DV, DN_HEADS * DN_DV, DN_HEADS, DN_HEADS,
            SA_WIDTH, SA_WIDTH, SA_WIDTH, IDX_HEADS * IDX_HD, IDX_HD, IDX_HEADS,
            XA_WIDTH, N_BRANCH * D_MODEL)
N_IN = sum(IN_SIZES)

kernel_name = 'hybrid_gdn_dsa_memxattn_block'


def rmsnorm(x, g):
    xf = x.astype(jnp.float32)
    y = xf * lax.rsqrt(jnp.mean(xf * xf, axis=-1, keepdims=True) + NORM_EPS)
    return (y * g.astype(jnp.float32)).astype(x.dtype)


def l2norm(x):
    return x * lax.rsqrt(jnp.sum(x * x, axis=-1, keepdims=True) + NORM_EPS)


def rope_partial(x, positions):
    rd = x.shape[-1] // ROPE_DIV
    half = rd // 2
    inv_freq = ROPE_THETA ** (-(jnp.arange(half, dtype=jnp.float32) * 2.0 / rd))
    ang = positions.astype(jnp.float32)[..., None] * inv_freq
    cos = jnp.cos(ang)[:, :, None, :]
    sin = jnp.sin(ang)[:, :, None, :]
    xr = x[..., :rd].astype(jnp.float32)
    x1, x2 = xr[..., :half], xr[..., half:]
    rot = jnp.concatenate([x1 * cos - x2 * sin, x2 * cos + x1 * sin], axis=-1)
    return jnp.concatenate([rot.astype(x.dtype), x[..., rd:]], axis=-1)


def causal_conv(x, w):
    c = x.shape[-1]
    return lax.conv_general_dilated(x, w[:, None, :], window_strides=(1,),
                                    padding=[(CONV_K - 1, 0)],
                                    dimension_numbers=('NWC', 'WIO', 'NWC'),
                                    feature_group_count=c)


def gated_delta_rule(q, k, v, beta, g):
    b, s, h, dk = q.shape
    dv = v.shape[-1]
    n = s // CHUNK

    def chunks(t):
        return jnp.moveaxis(t.reshape((b, n, CHUNK) + t.shape[2:]), 2, 3)

    qc, kc, vc, bc, gc = (chunks(t) for t in (q, k, v, beta, g))
    gc = jnp.cumsum(gc, axis=-1)
    pos = jnp.arange(CHUNK)
    incl = pos[:, None] >= pos[None, :]
    strict = pos[:, None] > pos[None, :]
    diff = gc[..., :, None] - gc[..., None, :]
    decay = jnp.where(incl, jnp.exp(jnp.where(incl, diff, 0.0)), 0.0)
    kb = kc * bc[..., None]
    m = jnp.where(strict, jnp.einsum('bnhid,bnhjd->bnhij', kb, kc) * decay, 0.0)
    rhs = jnp.concatenate([vc * bc[..., None], kb * jnp.exp(gc)[..., None]], axis=-1)
    sol = lax.linalg.triangular_solve(m, rhs, left_side=True, lower=True, unit_diagonal=True)
    u, w = sol[..., :dv], sol[..., dv:]
    a_qk = jnp.einsum('bnhid,bnhjd->bnhij', qc, kc) * decay
    q_dec = qc * jnp.exp(gc)[..., None]
    g_last = gc[..., -1]
    k_dec = kc * jnp.exp(g_last[..., None] - gc)[..., None]
    c_dec = jnp.exp(g_last)

    def step(state, xs):
        qd, kd, a, uu, ww, cd = xs
        v_new = uu - jnp.einsum('bhck,bhkv->bhcv', ww, state)
        o = jnp.einsum('bhck,bhkv->bhcv', qd, state) + jnp.einsum('bhij,bhjv->bhiv', a, v_new)
        state = state * cd[..., None, None] + jnp.einsum('bhck,bhcv->bhkv', kd, v_new)
        return state, o

    xs = tuple(jnp.moveaxis(t, 1, 0) for t in (q_dec, k_dec, a_qk, u, w, c_dec))
    s0 = jnp.zeros((b, h, dk, dv), jnp.float32)
    _, o = lax.scan(step, s0, xs)
    return jnp.swapaxes(jnp.moveaxis(o, 0, 1), 2, 3).reshape(b, s, h, dv)


def deltanet_branch(q_in, k_in, v_in, z_in, b_in, a_in, conv_w, a_log, dt_bias, dn_norm):
    b, s, _ = q_in.shape
    f32 = jnp.float32
    qkv = jax.nn.silu(causal_conv(jnp.concatenate([q_in, k_in, v_in], axis=-1), conv_w))
    q, k, v = jnp.split(qkv, [DN_WIDTH, 2 * DN_WIDTH], axis=-1)
    q = l2norm(q.reshape(b, s, DN_HEADS, DN_DK).astype(f32)) * DN_DK ** -0.5
    k = l2norm(k.reshape(b, s, DN_HEADS, DN_DK).astype(f32))
    v = v.reshape(b, s, DN_HEADS, DN_DV).astype(f32)
    beta = jax.nn.sigmoid(b_in.astype(f32))
    g = -jnp.exp(a_log.astype(f32)) * jax.nn.softplus(a_in.astype(f32) + dt_bias.astype(f32))
    o = gated_delta_rule(q, k, v, beta, g)
    z = z_in.reshape(b, s, DN_HEADS, DN_DV).astype(f32)
    o = rmsnorm(o, dn_norm) * jax.nn.silu(z)
    return o.reshape(b, s, DN_WIDTH).astype(q_in.dtype)


def dsa_branch(q_in, k_in, v_in, iq_in, ik_in, iw_in, positions):
    b, s, _ = q_in.shape
    f32 = jnp.float32
    q = rope_partial(q_in.reshape(b, s, SA_HEADS, SA_HD), positions)
    k = rope_partial(k_in.reshape(b, s, SA_HEADS, SA_HD), positions)
    v = v_in.reshape(b, s, SA_HEADS, SA_HD)
    iq = rope_partial(iq_in.reshape(b, s, IDX_HEADS, IDX_HD), positions).astype(f32)
    ik = rope_partial(ik_in.reshape(b, s, 1, IDX_HD), positions)[:, :, 0].astype(f32)
    iw = iw_in.astype(f32) * (IDX_HEADS ** -0.5 * IDX_HD ** -0.5)
    k_sel = min(TOPK_MAX, s // 4)
    nb = s // Q_BLOCK
    key_chunk = jnp.arange(s) // CHUNK

    def blocks(t):
        return jnp.moveaxis(t.reshape((b, nb, Q_BLOCK) + t.shape[2:]), 1, 0)

    def attend(xs):
        qb, iqb, iwb, blk = xs
        q_chunk = (blk * Q_BLOCK + jnp.arange(Q_BLOCK)) // CHUNK
        score = jax.nn.relu(jnp.einsum('bthd,bsd->bths', iqb, ik))
        score = jnp.einsum('bths,bth->bts', score, iwb)
        admissible = key_chunk[None, :] <= q_chunk[:, None]
        score = jnp.where(admissible[None], score, -jnp.inf)
        _, sel = lax.top_k(score, k_sel)
        valid = (sel // CHUNK) <= q_chunk[None, :, None]
        kg = jax.vmap(lambda a, i: a[i])(k, sel)
        vg = jax.vmap(lambda a, i: a[i])(v, sel)
        logits = jnp.einsum('bthd,btkhd->bthk', qb, kg).astype(f32) * SA_HD ** -0.5
        logits = jnp.where(valid[:, :, None, :], logits, -jnp.inf)
        p = jax.nn.softmax(logits, axis=-1).astype(vg.dtype)
        return jnp.einsum('bthk,btkhd->bthd', p, vg)

    o = lax.map(attend, (blocks(q), blocks(iq), blocks(iw), jnp.arange(nb, dtype=jnp.int32)))
    return jnp.moveaxis(o, 0, 1).reshape(b, s, SA_WIDTH)


def memory_branch(q_in, mem_n, w_mem_kv):
    b, s, _ = q_in.shape
    m = mem_n.shape[1]
    mk, mv = jnp.split(mem_n @ w_mem_kv, 2, axis=-1)
    q = q_in.reshape(b, s, XA_HEADS, XA_HD)
    mk = mk.reshape(b, m, XA_HEADS, XA_HD)
    mv = mv.reshape(b, m, XA_HEADS, XA_HD)
    logits = jnp.einsum('bshd,bmhd->bhsm', q, mk).astype(jnp.float32) * XA_HD ** -0.5
    p = jax.nn.softmax(logits, axis=-1).astype(mv.dtype)
    return jnp.einsum('bhsm,bmhd->bshd', p, mv).reshape(b, s, XA_WIDTH)


def setup_inputs(seed: int = 0) -> dict:
    key = jax.random.key(seed)
    ks = jax.random.split(key, 20)
    f32 = jnp.float32

    def nrm(k, shape, fan_in):
        return jax.random.normal(k, shape, f32) * fan_in ** -0.5

    def gain(k, shape):
        return 1.0 + 0.02 * jax.random.normal(k, shape, f32)

    x = jax.random.normal(ks[0], (BATCH, SEQ, D_MODEL), f32)
    mem = jax.random.normal(ks[1], (BATCH, N_MEM, D_MODEL), f32)
    offset = jax.random.randint(ks[2], (BATCH, 1), 0, 64, dtype=jnp.int32) * CHUNK
    positions = (offset + jnp.arange(SEQ, dtype=jnp.int32)[None, :]).astype(jnp.int32)
    dt = jnp.exp(jax.random.uniform(ks[3], (DEPTH, DN_HEADS), f32, math.log(1e-3), math.log(1e-1)))
    return {
        'x': x,
        'mem': mem,
        'positions': positions,
        'norm_mix': gain(ks[4], (DEPTH, D_MODEL)),
        'norm_mem': gain(ks[5], (DEPTH, D_MODEL)),
        'w_in': nrm(ks[6], (DEPTH, D_MODEL, N_IN), D_MODEL),
        'b_gate': 0.1 * jax.random.normal(ks[7], (DEPTH, N_BRANCH, D_MODEL), f32),
        'conv_w': nrm(ks[8], (DEPTH, CONV_K, 3 * DN_WIDTH), CONV_K),
        'a_log': jnp.log(jax.random.uniform(ks[9], (DEPTH, DN_HEADS), f32, 1.0, 16.0)),
        'dt_bias': jnp.log(jnp.expm1(dt)),
        'dn_norm': gain(ks[10], (DEPTH, DN_DV)),
        'w_mem_kv': nrm(ks[11], (DEPTH, D_MODEL, 2 * XA_WIDTH), D_MODEL),
        'w_branch': nrm(ks[12], (DEPTH, N_BRANCH, DN_WIDTH, D_MODEL), DN_WIDTH),
        'w_out': nrm(ks[13], (DEPTH, D_MODEL, D_MODEL), D_MODEL),
        'norm_ffn': gain(ks[14], (DEPTH, D_MODEL)),
        'w_ffn_in': nrm(ks[15], (DEPTH, D_MODEL, 2 * D_FF), D_MODEL),
        'w_ffn_out': nrm(ks[16], (DEPTH, D_FF, D_MODEL), D_FF),
        'norm_final': gain(ks[17], (D_MODEL,)),
    }


def reference(x, mem, positions, norm_mix, norm_mem, w_in, b_gate, conv_w, a_log, dt_bias,
              dn_norm, w_mem_kv, w_branch, w_out, norm_ffn, w_ffn_in, w_ffn_out, norm_final):
    b, s, d = x.shape
    split_pts = []
    acc = 0
    for size in IN_SIZES[:-1]:
        acc += size
        split_pts.append(acc)
    for l in range(DEPTH):
        h = rmsnorm(x, norm_mix[l])
        (dq, dk, dv, dz, db, da, sq, sk, sv, iq, ik, iw, xq, gl) = jnp.split(h @ w_in[l], split_pts, axis=-1)
        o_dn = deltanet_branch(dq, dk, dv, dz, db, da, conv_w[l], a_log[l], dt_bias[l], dn_norm[l])
        o_sa = dsa_branch(sq, sk, sv, iq, ik, iw, positions)
        o_xa = memory_branch(xq, rmsnorm(mem, norm_mem[l]), w_mem_kv[l])
        gates = jax.nn.sigmoid((gl.reshape(b, s, N_BRANCH, d) + b_gate[l]).astype(jnp.float32)).astype(x.dtype)
        merged = (gates[:, :, 0] * (o_dn @ w_branch[l, 0])
                  + gates[:, :, 1] * (o_sa @ w_branch[l, 1])
                  + gates[:, :, 2] * (o_xa @ w_branch[l, 2]))
        x = x + merged @ w_out[l]
        h = rmsnorm(x, norm_ffn[l])
        gt, up = jnp.split(h @ w_ffn_in[l], 2, axis=-1)
        x = x + (jax.nn.silu(gt) * up) @ w_ffn_out[l]
    return rmsnorm(x, norm_final)
```

```python
import math
import ml_dtypes
import numpy as np
import concourse.bass as bass
import concourse.mybir as mybir
from concourse.bass_utils import run_bass_kernel_spmd
from contextlib import ExitStack

F32 = mybir.dt.float32
BF16 = mybir.dt.bfloat16
I32 = mybir.dt.int32
AF = mybir.ActivationFunctionType
ALU = mybir.AluOpType
AX = mybir.AxisListType

SAME_ENGINE_WAIT = True


class T:
    def __init__(self, kb, t, name):
        self.kb = kb
        self.t = t
        self.name = name
        self.writer = None
        self.readers = []
        self.dsem = None
        self.dcnt = 0
        self.dma_pending_w = False
        self.dma_pending_r = False
        self.root = self

    def __getitem__(self, idx):
        return self.t[idx]


class KB:
    def __init__(self, nc, stack):
        self.nc = nc
        self.stack = stack
        self.engs = {'pe': nc.tensor, 'dve': nc.vector, 'act': nc.scalar, 'pool': nc.gpsimd, 'sp': nc.sync}
        self.sem = {n: stack.enter_context(nc.semaphore('prog_' + n)) for n in self.engs}
        self.cnt = {n: 0 for n in self.engs}
        self.waited = {n: {} for n in self.engs}
        self.ntile = 0

    def sb(self, shape, dt=F32, name=None):
        self.ntile += 1
        name = name or f"t{self.ntile}"
        t = self.stack.enter_context(self.nc.sbuf_tensor(name, list(shape), dt))
        return T(self, t, name)

    def ps(self, shape, dt=F32, name=None):
        self.ntile += 1
        name = name or f"p{self.ntile}"
        t = self.stack.enter_context(self.nc.psum_tensor(name, list(shape), dt))
        return T(self, t, name)

    def view(self, tile, lo, hi, name):
        v = T(self, tile.t[:, lo:hi], name)
        v.root = tile.root
        return v

    def _dsem(self, tile):
        if tile.dsem is None:
            tile.dsem = self.stack.enter_context(self.nc.semaphore('d_' + tile.name))
        return tile.dsem

    def _wait(self, eng, key, sem, val):
        w = self.waited[eng]
        if w.get(key, 0) < val:
            self.engs[eng].wait_ge(sem, val)
            w[key] = val

    def _deps(self, eng, reads, writes):
        deps = {}
        reads = [t.root for t in reads]
        writes = [t.root for t in writes]

        def add(e, i):
            if e == eng and not SAME_ENGINE_WAIT:
                return
            deps[e] = max(deps.get(e, 0), i)

        for t in reads:
            if t.writer:
                add(*t.writer)
            if t.dsem is not None and t.dma_pending_w:
                self._wait(eng, ('d', t.name), t.dsem, 16 * t.dcnt)
        for t in writes:
            if t.writer:
                add(*t.writer)
            for r in t.readers:
                add(*r)
            if t.dsem is not None and (t.dma_pending_w or t.dma_pending_r):
                self._wait(eng, ('d', t.name), t.dsem, 16 * t.dcnt)
        for e, i in deps.items():
            self._wait(eng, e, self.sem[e], i)

    def op(self, eng, fn, reads=(), writes=()):
        self._deps(eng, reads, writes)
        inst = fn(self.engs[eng])
        inst.then_inc(self.sem[eng], 1)
        self.cnt[eng] += 1
        me = (eng, self.cnt[eng])
        reads = [t.root for t in reads]
        writes = [t.root for t in writes]
        for t in reads:
            t.readers.append(me)
            if len(t.readers) > 64:
                t.readers = self._prune(t.readers)
        for t in writes:
            t.writer = me
            t.readers = []
            t.dma_pending_w = False
            t.dma_pending_r = False
        return inst

    @staticmethod
    def _prune(rs):
        best = {}
        for e, i in rs:
            best[e] = max(best.get(e, 0), i)
        return list(best.items())

    def dma(self, out, in_, tile, is_load, q='sp', **kw):
        if is_load:
            self._deps(q, (), (tile,))
        else:
            self._deps(q, (tile,), ())
        sem = self._dsem(tile)
        inst = self.engs[q].dma_start(out=out, in_=in_, **kw)
        inst.then_inc(sem, 16)
        tile.dcnt += 1
        if is_load:
            tile.writer = None
            tile.readers = []
            tile.dma_pending_w = True
        else:
            tile.dma_pending_r = True
        return inst

    def finish(self, tiles, eng='sp'):
        for t in tiles:
            if t.dsem is not None:
                self.engs[eng].wait_ge(t.dsem, 16 * t.dcnt)


D = 1024
N_IN = 11864
SEGS = [('dq', 0, 1024), ('dk', 1024, 1024), ('dv', 2048, 1024), ('dz', 3072, 1024), ('dba', 4096, 16),
        ('sq', 4112, 1024), ('sk', 5136, 1024), ('sv', 6160, 1024), ('iq', 7184, 512), ('ikw', 7696, 72),
        ('xq', 7768, 1024), ('gl', 8792, 3072)]
TWO_PI = 2.0 * math.pi


def rope_tables(kb, posf, invf, nt, half, name):
    ang = kb.sb([128, nt, half], F32, name + '_ang')
    kb.op('dve', lambda e: e.tensor_tensor(out=ang[:], in0=invf[:].unsqueeze(1).to_broadcast([128, nt, half]),
                                           in1=posf[:].unsqueeze(2).to_broadcast([128, nt, half]), op=ALU.mult),
          reads=[posf, invf], writes=[ang])
    r = kb.sb([128, nt, half], F32, name + '_r')
    kb.op('dve', lambda e: e.tensor_scalar(out=r[:], in0=ang[:], scalar1=1.0 / TWO_PI, scalar2=None, op0=ALU.mult),
          reads=[ang], writes=[r])
    ni = kb.sb([128, nt, half], I32, name + '_ni')
    kb.op('dve', lambda e: e.tensor_copy(out=ni[:], in_=r[:]), reads=[r], writes=[ni])
    nf = kb.sb([128, nt, half], F32, name + '_nf')
    kb.op('dve', lambda e: e.tensor_copy(out=nf[:], in_=ni[:]), reads=[ni], writes=[nf])
    fr = kb.sb([128, nt, half], F32, name + '_fr')
    kb.op('dve', lambda e: e.tensor_tensor(out=fr[:], in0=r[:], in1=nf[:], op=ALU.subtract), reads=[r, nf], writes=[fr])
    def wrapped(shift, nm2):
        f = kb.sb([128, nt, half], F32, name + nm2 + '_f')
        t = kb.sb([128, nt, half], F32, name + nm2 + '_t')
        kb.op('dve', lambda e: e.tensor_scalar(out=f[:], in0=fr[:], scalar1=shift, scalar2=None, op0=ALU.add),
              reads=[fr], writes=[f])
        for _ in range(2):
            kb.op('dve', lambda e: e.tensor_scalar(out=t[:], in0=f[:], scalar1=0.5, scalar2=-1.0, op0=ALU.is_gt, op1=ALU.mult),
                  reads=[f], writes=[t])
            kb.op('dve', lambda e: e.tensor_tensor(out=f[:], in0=f[:], in1=t[:], op=ALU.add), reads=[f, t], writes=[f])
        kb.op('dve', lambda e: e.tensor_scalar(out=t[:], in0=f[:], scalar1=-0.5, scalar2=None, op0=ALU.is_lt),
              reads=[f], writes=[t])
        kb.op('dve', lambda e: e.tensor_tensor(out=f[:], in0=f[:], in1=t[:], op=ALU.add), reads=[f, t], writes=[f])
        kb.op('dve', lambda e: e.tensor_scalar(out=f[:], in0=f[:], scalar1=TWO_PI, scalar2=3.14159, op0=ALU.mult, op1=ALU.min),
              reads=[f], writes=[f])
        kb.op('dve', lambda e: e.tensor_scalar(out=f[:], in0=f[:], scalar1=-3.14159, scalar2=None, op0=ALU.max),
              reads=[f], writes=[f])
        o = kb.sb([128, nt, half], F32, name + nm2)
        kb.op('act', lambda e: e.activation(out=o[:], in_=f[:], func=AF.Sin), reads=[f], writes=[o])
        return o
    sin = wrapped(0.0, '_sin')
    cos = wrapped(0.25, '_cos')
    return cos, sin


def make_ident(kb, dt=F32, name='ident'):
    ones = kb.sb([128, 128], F32, name + '_ones')
    ident = kb.sb([128, 128], dt, name)
    kb.op('pool', lambda e: e.memset(ones[:], 1.0), writes=[ones])
    kb.op('pool', lambda e: e.affine_select(out=ident[:], in_=ones[:], pattern=[[-1, 128]], compare_op=ALU.is_equal,
                                            fill=0.0, base=0, channel_multiplier=1), reads=[ones], writes=[ident])
    return ident, ones


def build_A(NT=4096, with_rope=True):
    nt = NT // 128
    nc = bass.Bass("TRN2", target_bir_lowering=False)
    x = nc.dram_tensor("x", [NT, D], F32, kind="ExternalInput").ap()
    pos = nc.dram_tensor("pos", [128, nt], I32, kind="ExternalInput").ap()
    gcol = nc.dram_tensor("gcol", [128, 8], F32, kind="ExternalInput").ap()
    w = nc.dram_tensor("w", [D, N_IN], F32, kind="ExternalInput").ap()
    invf_sa = nc.dram_tensor("invf_sa", [128, 16], F32, kind="ExternalInput").ap()
    invf_ix = nc.dram_tensor("invf_ix", [128, 8], F32, kind="ExternalInput").ap()
    P = nc.dram_tensor("P", [NT, N_IN], F32, kind="ExternalOutput").ap()
    Pb = nc.dram_tensor("Pb", [NT, 3072], BF16, kind="ExternalOutput").ap()
    with ExitStack() as st:
        kb = KB(nc, st)
        ident, _ = make_ident(kb)
        g_sb = kb.sb([128, 8], F32, 'g_sb')
        kb.dma(g_sb[:], gcol[:, :], g_sb, True)
        posi = kb.sb([128, nt], I32, 'posi')
        kb.dma(posi[:], pos[:, :], posi, True)
        posf = kb.sb([128, nt], F32, 'posf')
        kb.op('dve', lambda e: e.tensor_copy(out=posf[:], in_=posi[:]), reads=[posi], writes=[posf])
        ifs = kb.sb([128, 16], F32, 'ifs')
        ifx = kb.sb([128, 8], F32, 'ifx')
        kb.dma(ifs[:], invf_sa[:, :], ifs, True)
        kb.dma(ifx[:], invf_ix[:, :], ifx, True)
        cos_sa, sin_sa = rope_tables(kb, posf, ifs, nt, 16, 'rsa')
        cos_ix, sin_ix = rope_tables(kb, posf, ifx, nt, 8, 'rix')

        hT = kb.sb([128, 8, NT], BF16, 'hT')
        xts = [kb.sb([128, D], F32, f'xt{i}') for i in range(2)]
        xns = [kb.sb([128, D], F32, f'xn{i}') for i in range(2)]
        junk = kb.sb([128, D], F32, 'junk')
        sss = [kb.sb([128, 1], F32, f'ss{i}') for i in range(2)]
        rss = [kb.sb([128, 1], F32, f'rs{i}') for i in range(2)]
        ptr = [kb.ps([128, 512], F32, f'ptr{i}') for i in range(2)]
        pmm = [kb.ps([128, 512], F32, f'pmm{i}') for i in range(4)]
        for i in range(nt):
            xt, xn, ss, rs = xts[i % 2], xns[i % 2], sss[i % 2], rss[i % 2]
            kb.dma(xt[:], x[i * 128:(i + 1) * 128, :], xt, True)
            kb.op('act', lambda e: e.activation(out=junk[:], in_=xt[:], func=AF.Square, accum_out=ss[:]),
                  reads=[xt], writes=[junk, ss])
            kb.op('act', lambda e: e.activation(out=rs[:], in_=ss[:], func=AF.Sqrt, scale=1.0 / D, bias=1e-6),
                  reads=[ss], writes=[rs])
            kb.op('dve', lambda e: e.reciprocal(out=rs[:], in_=rs[:]), reads=[rs], writes=[rs])
            kb.op('dve', lambda e: e.tensor_scalar(out=xn[:], in0=xt[:], scalar1=rs[:, 0:1], scalar2=None, op0=ALU.mult),
                  reads=[xt, rs], writes=[xn])
            for half in range(2):
                pt = ptr[half]
                for j in range(4):
                    kc = half * 4 + j
                    kb.op('pe', lambda e: e.transpose(out=pt[:, j * 128:(j + 1) * 128], in_=xn[:, kc * 128:(kc + 1) * 128],
                                                      identity=ident[:]), reads=[xn, ident], writes=[pt])
                eng = 'act' if half == 0 else 'dve'
                dst = hT[:, half * 4:(half + 1) * 4, i * 128:(i + 1) * 128]
                src = pt[:].rearrange("p (a b) -> p a b", a=4)
                if eng == 'act':
                    kb.op('act', lambda e: e.activation(out=dst, in_=src, func=AF.Copy), reads=[pt], writes=[hT])
                else:
                    kb.op('dve', lambda e: e.tensor_copy(out=dst, in_=src), reads=[pt], writes=[hT])

        chunks = []
        for (nm, c0, wd) in SEGS:
            o = 0
            while o < wd:
                cw = min(512, wd - o)
                chunks.append((nm, c0 + o, cw, o))
                o += cw
        wf = [kb.sb([128, 8, 512], F32, f'wf{i}') for i in range(2)]
        wb = [kb.sb([128, 8, 512], BF16, f'wb{i}') for i in range(2)]
        stg = [kb.sb([128, 512], F32, f'stg{i}') for i in range(4)]
        stb = [kb.sb([128, 512], BF16, f'stb{i}') for i in range(2)]
        rt = [kb.sb([128, 64], F32, f'rt{i}') for i in range(6)]
        nev = 0
        for ci, (nm, c0, cw, soff) in enumerate(chunks):
            wfc, wbc = wf[ci % 2], wb[ci % 2]
            kb.dma(wfc[:, :, 0:cw], w[:, c0:c0 + cw].rearrange("(kc k) n -> k kc n", k=128), wfc, True)
            for kc in range(8):
                kb.op('pool', lambda e: e.tensor_scalar(out=wbc[:, kc, 0:cw], in0=wfc[:, kc, 0:cw], scalar1=g_sb[:, kc:kc + 1],
                                                        scalar2=None, op0=ALU.mult), reads=[wfc, g_sb], writes=[wbc])
            for i in range(nt):
                pm = pmm[nev % 4]
                for kc in range(8):
                    kb.op('pe', lambda e: e.matmul(pm[:, 0:cw], lhsT=hT[:, kc, i * 128:(i + 1) * 128], rhs=wbc[:, kc, 0:cw],
                                                   start=(kc == 0), stop=(kc == 7)), reads=[hT, wbc], writes=[pm])
                sg = stg[nev % 4]
                if nev % 2 == 0:
                    kb.op('act', lambda e: e.activation(out=sg[:, 0:cw], in_=pm[:, 0:cw], func=AF.Copy), reads=[pm], writes=[sg])
                else:
                    kb.op('dve', lambda e: e.tensor_copy(out=sg[:, 0:cw], in_=pm[:, 0:cw]), reads=[pm], writes=[sg])
                nev += 1
                if with_rope and nm in ('sq', 'sk', 'iq', 'ikw'):
                    if nm in ('sq', 'sk'):
                        nh, hd, hf, cs, sn = 4, 128, 16, cos_sa, sin_sa
                    elif nm == 'iq':
                        nh, hd, hf, cs, sn = 8, 64, 8, cos_ix, sin_ix
                    else:
                        nh, hd, hf, cs, sn = 1, 64, 8, cos_ix, sin_ix
                    v = sg[:, 0:nh * hd].rearrange("p (h d) -> p h d", h=nh)
                    x1, x2 = v[:, :, 0:hf], v[:, :, hf:2 * hf]
                    cb = cs[:, i, :].unsqueeze(1).to_broadcast([128, nh, hf])
                    sb_ = sn[:, i, :].unsqueeze(1).to_broadcast([128, nh, hf])
                    tt = [rt[k][:, 0:nh * hf].rearrange("p (h d) -> p h d", h=nh) for k in range(6)]
                    for k, (a, b) in enumerate([(x1, cb), (x2, sb_), (x2, cb), (x1, sb_)]):
                        kb.op('pool', lambda e: e.tensor_tensor(out=tt[k], in0=a, in1=b, op=ALU.mult),
                              reads=[sg, cs, sn], writes=[rt[k]])
                    kb.op('pool', lambda e: e.tensor_tensor(out=x1, in0=tt[0], in1=tt[1], op=ALU.subtract),
                          reads=[rt[0], rt[1]], writes=[sg])
                    kb.op('pool', lambda e: e.tensor_tensor(out=x2, in0=tt[2], in1=tt[3], op=ALU.add),
                          reads=[rt[2], rt[3]], writes=[sg])
                kb.dma(P[i * 128:(i + 1) * 128, c0:c0 + cw], sg[:, 0:cw], sg, False)
                if nm in ('sq', 'sk', 'sv'):
                    bo = {'sq': 0, 'sk': 1024, 'sv': 2048}[nm] + soff
                    sbt = stb[i % 2]
                    kb.op('pool', lambda e: e.tensor_copy(out=sbt[:, 0:cw], in_=sg[:, 0:cw]), reads=[sg], writes=[sbt])
                    kb.dma(Pb[i * 128:(i + 1) * 128, bo:bo + cw], sbt[:, 0:cw], sbt, False)
        kb.finish(stg + stb)
    return nc


def host_consts():
    inv_sa = (500000.0 ** (-(np.arange(16, dtype=np.float32) * 2.0 / 32))).astype(np.float32)
    inv_ix = (500000.0 ** (-(np.arange(8, dtype=np.float32) * 2.0 / 16))).astype(np.float32)
    return np.tile(inv_sa[None], (128, 1)), np.tile(inv_ix[None], (128, 1))


S_LEN = 16384
NBLK = S_LEN // 128
NEG = -30000.0


def build_B(NH=2, nblk=NBLK, NB=4, stage=9, sub=9):
    SL = nblk * 128
    nc = bass.Bass("TRN2", target_bir_lowering=False)
    xp = nc.dram_tensor("xp", [NH, SL + 3, 384], F32, kind="ExternalInput").ap()
    zin = nc.dram_tensor("z", [NH, SL, 128], F32, kind="ExternalInput").ap()
    bain = nc.dram_tensor("ba", [NH, 128, 2, nblk], F32, kind="ExternalInput").ap()
    wcin = nc.dram_tensor("wc", [NH, 128, 4, 384], F32, kind="ExternalInput").ap()
    hpin = nc.dram_tensor("hp", [NH, 128, 2], F32, kind="ExternalInput").ap()
    dnin = nc.dram_tensor("dnw", [128, 128], F32, kind="ExternalInput").ap()
    oout = nc.dram_tensor("o", [NH, SL, 128], F32, kind="ExternalOutput").ap()
    with ExitStack() as st:
        kb = KB(nc, st)
        ident, ones = make_ident(kb)
        def tri_mask(name, allowed_fill, other_fill, kind):
            m = kb.sb([128, 128], F32, name)
            kb.op('pool', lambda e: e.memset(m[:], allowed_fill), writes=[m])
            if kind == 'upper_incl':
                kb.op('pool', lambda e: e.affine_select(out=m[:], in_=m[:], pattern=[[1, 128]], compare_op=ALU.is_ge,
                                                        fill=other_fill, base=0, channel_multiplier=-1), reads=[m], writes=[m])
                kb.op('pool', lambda e: e.memset(m[0:64, 64:128], other_fill), writes=[m])
            else:
                kb.op('pool', lambda e: e.affine_select(out=m[:], in_=m[:], pattern=[[-1, 128]], compare_op=ALU.is_ge,
                                                        fill=other_fill, base=-1, channel_multiplier=1), reads=[m], writes=[m])
                kb.op('pool', lambda e: e.memset(m[64:128, 0:64], other_fill), writes=[m])
            return m
        U2 = tri_mask('U2', 1.0, 0.0, 'upper_incl')
        NEGUI = tri_mask('NEGUI', 0.0, NEG, 'upper_incl')
        NEGL = tri_mask('NEGL', 0.0, NEG, 'lower_strict')
        dnw = kb.sb([128, 128], F32, 'dnw_sb')
        kb.dma(dnw[:], dnin[:, :], dnw, True)

        banks = [kb.ps([128, 512], F32, f'bank{i}') for i in range(8)]
        pT = banks[0]
        pk = kb.view(banks[1], 0, 384, 'pk')
        ppow = [kb.view(banks[2], 0, 256, 'ppow0'), kb.view(banks[3], 0, 256, 'ppow1')]
        pR = [kb.view(banks[4], 0, 128, 'pR0'), kb.view(banks[5], 0, 128, 'pR1')]
        pwa = kb.view(banks[4], 128, 256, 'pwa')
        po2 = [kb.view(banks[4], 256, 384, 'po0'), kb.view(banks[4], 384, 512, 'po1')]
        pg = kb.view(banks[5], 128, 256, 'pg')
        pau = kb.view(banks[5], 256, 384, 'pau')
        pss = kb.view(banks[6], 0, 128, 'pss')
        pa = kb.view(banks[6], 128, 256, 'pa')
        pbig = banks[7]
        puw = kb.view(banks[7], 0, 256, 'puw')

        def alloc(n, shape, name):
            return [kb.sb(shape, F32, f'{name}{i}') for i in range(n)]

        for hh in range(NH):
            ba = kb.sb([128, 2, nblk], F32, f'ba{hh}')
            kb.dma(ba[:], bain[hh], ba, True)
            hp = kb.sb([128, 2], F32, f'hp{hh}')
            kb.dma(hp[:], hpin[hh], hp, True)
            wc = kb.sb([128, 4, 384], F32, f'wc{hh}')
            kb.dma(wc[:], wcin[hh], wc, True)
            beta = kb.sb([128, nblk], F32, f'beta{hh}')
            kb.op('act', lambda e: e.activation(out=beta[:], in_=ba[:, 0, :], func=AF.Sigmoid), reads=[ba], writes=[beta])
            nega = kb.sb([128, 1], F32, f'nega{hh}')
            kb.op('act', lambda e: e.activation(out=nega[:], in_=hp[:, 0:1], func=AF.Exp), reads=[hp], writes=[nega])
            kb.op('dve', lambda e: e.tensor_scalar(out=nega[:], in0=nega[:], scalar1=-1.0, scalar2=None, op0=ALU.mult),
                  reads=[nega], writes=[nega])
            g = kb.sb([128, nblk], F32, f'g{hh}')
            kb.op('act', lambda e: e.activation(out=g[:], in_=ba[:, 1, :], func=AF.Exp, bias=hp[:, 1:2]), reads=[ba, hp], writes=[g])
            kb.op('act', lambda e: e.activation(out=g[:], in_=g[:], func=AF.Ln, bias=1.0), reads=[g], writes=[g])
            kb.op('dve', lambda e: e.tensor_scalar(out=g[:], in0=g[:], scalar1=nega[:, 0:1], scalar2=None, op0=ALU.mult),
                  reads=[g, nega], writes=[g])
            kb.op('pe', lambda e: e.matmul(pbig[:, 0:nblk], lhsT=U2[:], rhs=g[:], start=True, stop=True), reads=[U2, g], writes=[pbig])
            gc = kb.sb([128, nblk], F32, f'gc{hh}')
            kb.op('dve', lambda e: e.tensor_copy(out=gc[:], in_=pbig[:, 0:nblk]), reads=[pbig], writes=[gc])
            g2 = kb.sb([128, 2, nblk], F32, f'g2{hh}')
            kb.op('pool', lambda e: e.memset(g2[:], 0.0), writes=[g2])
            kb.op('pool', lambda e: e.tensor_copy(out=g2[0:64, 0, :], in_=g[0:64, :]), reads=[g], writes=[g2])
            kb.op('pool', lambda e: e.tensor_copy(out=g2[64:128, 1, :], in_=g[64:128, :]), reads=[g], writes=[g2])
            kb.op('pe', lambda e: e.matmul(pbig[:, 0:2 * nblk], lhsT=ones[:], rhs=g2[:].rearrange("p a b -> p (a b)"),
                                           start=True, stop=True), reads=[ones, g2], writes=[pbig])
            glB = kb.sb([128, 2, nblk], F32, f'glB{hh}')
            kb.op('dve', lambda e: e.tensor_copy(out=glB[:].rearrange("p a b -> p (a b)"), in_=pbig[:, 0:2 * nblk]),
                  reads=[pbig], writes=[glB])
            cd = kb.sb([128, 2, nblk], F32, f'cd{hh}')
            kb.op('act', lambda e: e.activation(out=cd[:], in_=glB[:], func=AF.Exp), reads=[glB], writes=[cd])
            ekd = kb.sb([128, nblk], F32, f'ekd{hh}')
            kb.op('dve', lambda e: e.tensor_tensor(out=ekd[0:64, :], in0=glB[0:64, 0, :], in1=gc[0:64, :], op=ALU.subtract),
                  reads=[glB, gc], writes=[ekd])
            kb.op('dve', lambda e: e.tensor_tensor(out=ekd[64:128, :], in0=glB[64:128, 1, :], in1=gc[64:128, :], op=ALU.subtract),
                  reads=[glB, gc], writes=[ekd])
            kb.op('act', lambda e: e.activation(out=ekd[:], in_=ekd[:], func=AF.Exp), reads=[ekd], writes=[ekd])
            eg = kb.sb([128, nblk], F32, f'eg{hh}')
            kb.op('act', lambda e: e.activation(out=eg[:], in_=gc[:], func=AF.Exp), reads=[gc], writes=[eg])
            beg = kb.sb([128, nblk], F32, f'beg{hh}')
            kb.op('dve', lambda e: e.tensor_tensor(out=beg[:], in0=beta[:], in1=eg[:], op=ALU.mult), reads=[beta, eg], writes=[beg])

            S = alloc(2, [128, 128], f'S{hh}_')
            kb.op('pool', lambda e: e.memset(S[0][:], 0.0), writes=[S[0]])
            scur = 0

            if hh == 0:
                X = alloc(4, [128, NB, 384], 'X')
                y = kb.sb([128, NB, 384], F32, 'y')
                tmp = kb.sb([128, NB, 384], F32, 'tmp')
                sq = kb.sb([128, NB, 2, 128], F32, 'sq')
                ssn = kb.sb([128, NB, 2], F32, 'ssn')
                zt = kb.sb([128, NB, 128], F32, 'zt')
                zw = kb.sb([128, NB, 128], F32, 'zw')
                kbt = kb.sb([128, NB, 128], F32, 'kbt')
                rw = kb.sb([128, NB, 256], F32, 'rw')
                kdt = kb.sb([128, NB, 128], F32, 'kdt')
                kdm = kb.sb([128, NB, 2, 128], F32, 'kdm')
                kb.op('pool', lambda e: e.memset(kdm[:], 0.0), writes=[kdm])
                qdt = kb.sb([128, NB, 128], F32, 'qdt')
                TT = alloc(NB, [128, 4, 128], 'TT')
                dg = alloc(NB, [128, 128], 'dg')
                aL = alloc(NB, [128, 128], 'aL')
                aU = alloc(NB, [128, 128], 'aU')
                DU = alloc(NB, [128, 128], 'DU')
                Mm = alloc(NB, [128, 128], 'Mm')
                Nm = alloc(NB, [128, 128], 'Nm')
                ATm = alloc(NB, [128, 128], 'ATm')
                Rm = [alloc(NB, [128, 128], f'R{k}_') for k in range(2)]
                PW = [alloc(NB, [128, 256], f'PW{k}_') for k in range(2)]
                uw = alloc(NB, [128, 256], 'uw')
                QpT = alloc(NB, [128, 128], 'QpT')
                au = alloc(NB, [128, 128], 'au')
                AcT = alloc(2 * NB, [128, 128], 'AcT')
                osb = alloc(NB, [128, 128], 'osb')
                junk = kb.sb([128, 128], F32, 'junk')
                oss = alloc(NB, [128, 1], 'oss')
                ofin = alloc(NB, [128, 128], 'ofin')

            for g0 in range(0, nblk if stage >= 2 else 0, NB):
                for j in range(4):
                    src = xp[hh, j + g0 * 128: j + (g0 + NB) * 128, :].rearrange("(b p) c -> p b c", p=128)
                    kb.dma(X[j][:], src, X[j], True)
                kb.dma(zt[:], zin[hh, g0 * 128:(g0 + NB) * 128, :].rearrange("(b p) c -> p b c", p=128), zt, True)
                def wb(j):
                    return wc[:, j, :].unsqueeze(1).to_broadcast([128, NB, 384])
                kb.op('dve', lambda e: e.tensor_tensor(out=y[:], in0=X[0][:], in1=wb(0), op=ALU.mult), reads=[X[0], wc], writes=[y])
                for j in range(1, 4):
                    kb.op('pool', lambda e: e.tensor_tensor(out=tmp[:], in0=X[j][:], in1=wb(j), op=ALU.mult), reads=[X[j], wc], writes=[tmp])
                    kb.op('dve', lambda e: e.tensor_tensor(out=y[:], in0=y[:], in1=tmp[:], op=ALU.add), reads=[y, tmp], writes=[y])
                kb.op('act', lambda e: e.activation(out=y[:], in_=y[:], func=AF.Silu), reads=[y], writes=[y])
                yv = y[:].rearrange("p b (t d) -> p b t d", t=3)
                kb.op('pool', lambda e: e.tensor_tensor(out=sq[:], in0=yv[:, :, 0:2, :], in1=yv[:, :, 0:2, :], op=ALU.mult), reads=[y], writes=[sq])
                kb.op('dve', lambda e: e.tensor_reduce(out=ssn[:], in_=sq[:], axis=AX.X, op=ALU.add), reads=[sq], writes=[ssn])
                kb.op('act', lambda e: e.activation(out=ssn[:], in_=ssn[:], func=AF.Sqrt, bias=1e-6), reads=[ssn], writes=[ssn])
                kb.op('dve', lambda e: e.reciprocal(out=ssn[:], in_=ssn[:]), reads=[ssn], writes=[ssn])
                kb.op('dve', lambda e: e.tensor_scalar(out=ssn[:, :, 0:1], in0=ssn[:, :, 0:1], scalar1=128.0 ** -0.5, scalar2=None, op0=ALU.mult),
                      reads=[ssn], writes=[ssn])
                kb.op('dve', lambda e: e.tensor_tensor(out=yv[:, :, 0:2, :], in0=yv[:, :, 0:2, :],
                                                       in1=ssn[:].unsqueeze(3).to_broadcast([128, NB, 2, 128]), op=ALU.mult),
                      reads=[y, ssn], writes=[y])
                qn, kn, vv = yv[:, :, 0, :], yv[:, :, 1, :], yv[:, :, 2, :]
                def bc(tab):
                    return tab[:, g0:g0 + NB].unsqueeze(2).to_broadcast([128, NB, 128])
                kb.op('pool', lambda e: e.tensor_tensor(out=kbt[:], in0=kn, in1=bc(beta), op=ALU.mult), reads=[y, beta], writes=[kbt])
                kb.op('dve', lambda e: e.tensor_tensor(out=rw[:, :, 0:128], in0=vv, in1=bc(beta), op=ALU.mult), reads=[y, beta], writes=[rw])
                kb.op('pool', lambda e: e.tensor_tensor(out=rw[:, :, 128:256], in0=kn, in1=bc(beg), op=ALU.mult), reads=[y, beg], writes=[rw])
                kb.op('dve', lambda e: e.tensor_tensor(out=kdt[:], in0=kn, in1=bc(ekd), op=ALU.mult), reads=[y, ekd], writes=[kdt])
                kb.op('pool', lambda e: e.tensor_tensor(out=qdt[:], in0=qn, in1=bc(eg), op=ALU.mult), reads=[y, eg], writes=[qdt])
                kb.op('pool', lambda e: e.tensor_copy(out=kdm[0:64, :, 0, :], in_=kdt[0:64, :, :]), reads=[kdt], writes=[kdm])
                kb.op('pool', lambda e: e.tensor_copy(out=kdm[64:128, :, 1, :], in_=kdt[64:128, :, :]), reads=[kdt], writes=[kdm])
                kb.op('act', lambda e: e.activation(out=zw[:], in_=zt[:], func=AF.Silu), reads=[zt], writes=[zw])
                kb.op('pool', lambda e: e.tensor_tensor(out=zw[:], in0=zw[:], in1=dnw[:].unsqueeze(1).to_broadcast([128, NB, 128]), op=ALU.mult),
                      reads=[zw, dnw], writes=[zw])
                for b in range(NB if stage >= 3 else 0):
                    blk = g0 + b
                    for j, (srcT, sap) in enumerate([(y, kn[:, b, :]), (kbt, kbt[:, b, :]), (y, qn[:, b, :]), (qdt, qdt[:, b, :])]):
                        kb.op('pe', lambda e: e.transpose(out=pT[:, j * 128:(j + 1) * 128], in_=sap, identity=ident[:]),
                              reads=[srcT, ident], writes=[pT])
                    kb.op('act', lambda e: e.activation(out=TT[b][:].rearrange("p a b -> p (a b)"), in_=pT[:], func=AF.Copy),
                          reads=[pT], writes=[TT[b]])
                    kT, kbT, qT, qdT = (TT[b][:, j, :] for j in range(4))
                    if sub < 2:
                        continue
                    kb.op('pool', lambda e: e.tensor_scalar(out=dg[b][:], in0=ident[:], scalar1=gc[:, blk:blk + 1], scalar2=None, op0=ALU.mult),
                          reads=[ident, gc], writes=[dg[b]])
                    kb.op('pe', lambda e: e.matmul(pg[:], lhsT=ones[:], rhs=dg[b][:], start=True, stop=True), reads=[ones, dg[b]], writes=[pg])
                    kb.op('dve', lambda e: e.tensor_scalar(out=aU[b][:], in0=pg[:], scalar1=gc[:, blk:blk + 1], scalar2=None, op0=ALU.subtract),
                          reads=[pg, gc], writes=[aU[b]])
                    kb.op('pool', lambda e: e.tensor_scalar(out=aL[b][:], in0=aU[b][:], scalar1=-1.0, scalar2=None, op0=ALU.mult),
                          reads=[aU[b]], writes=[aL[b]])
                    kb.op('pool', lambda e: e.tensor_tensor(out=aL[b][:], in0=aL[b][:], in1=NEGL[:], op=ALU.add), reads=[aL[b], NEGL], writes=[aL[b]])
                    kb.op('pool', lambda e: e.tensor_tensor(out=aU[b][:], in0=aU[b][:], in1=NEGUI[:], op=ALU.add), reads=[aU[b], NEGUI], writes=[aU[b]])
                    kb.op('act', lambda e: e.activation(out=aL[b][:], in_=aL[b][:], func=AF.Exp), reads=[aL[b]], writes=[aL[b]])
                    kb.op('act', lambda e: e.activation(out=aU[b][:], in_=aU[b][:], func=AF.Exp), reads=[aU[b]], writes=[aU[b]])
                    kb.op('pool', lambda e: e.tensor_tensor(out=DU[b][:], in0=aU[b][:], in1=ident[:], op=ALU.subtract), reads=[aU[b], ident], writes=[DU[b]])
                    if sub < 3:
                        continue
                    kb.op('pe', lambda e: e.matmul(pk[:, 0:128], lhsT=kbT, rhs=kT, start=True, stop=True), reads=[TT[b]], writes=[pk])
                    kb.op('pe', lambda e: e.matmul(pk[:, 128:256], lhsT=kT, rhs=kbT, start=True, stop=True), reads=[TT[b]], writes=[pk])
                    kb.op('pe', lambda e: e.matmul(pk[:, 256:384], lhsT=kT, rhs=qT, start=True, stop=True), reads=[TT[b]], writes=[pk])
                    if sub < 4:
                        continue
                    kb.op('dve', lambda e: e.tensor_tensor(out=Mm[b][:], in0=pk[:, 0:128], in1=aL[b][:], op=ALU.mult), reads=[pk, aL[b]], writes=[Mm[b]])
                    kb.op('dve', lambda e: e.tensor_tensor(out=Nm[b][:], in0=pk[:, 128:256], in1=DU[b][:], op=ALU.mult), reads=[pk, DU[b]], writes=[Nm[b]])
                    kb.op('dve', lambda e: e.tensor_tensor(out=ATm[b][:], in0=pk[:, 256:384], in1=aU[b][:], op=ALU.mult), reads=[pk, aU[b]], writes=[ATm[b]])
                    kb.op('pool', lambda e: e.tensor_tensor(out=Rm[0][b][:], in0=ident[:], in1=Nm[b][:], op=ALU.subtract), reads=[ident, Nm[b]], writes=[Rm[0][b]])
                curM = [Mm[b][:] for b in range(NB)]
                curN = [Nm[b][:] for b in range(NB)]
                curMt = [Mm[b] for b in range(NB)]
                curNt = [Nm[b] for b in range(NB)]
                rcur = 0
                for lvl in range(5 if stage >= 4 else 0):
                    last = (lvl == 4)
                    pw = PW[lvl % 2]
                    for b in range(NB):
                        pp = ppow[b % 2]
                        kb.op('pe', lambda e: e.matmul(pp[:, 0:128], lhsT=curN[b], rhs=curM[b], start=True, stop=True),
                              reads=[curMt[b], curNt[b]], writes=[pp])
                        if not last:
                            kb.op('pe', lambda e: e.matmul(pp[:, 128:256], lhsT=curM[b], rhs=curN[b], start=True, stop=True),
                                  reads=[curMt[b], curNt[b]], writes=[pp])
                        wd = 128 if last else 256
                        if b % 2 == 0:
                            kb.op('act', lambda e: e.activation(out=pw[b][:, 0:wd], in_=pp[:, 0:wd], func=AF.Copy), reads=[pp], writes=[pw[b]])
                        else:
                            kb.op('dve', lambda e: e.tensor_copy(out=pw[b][:, 0:wd], in_=pp[:, 0:wd]), reads=[pp], writes=[pw[b]])
                    for b in range(NB):
                        pr = pR[b % 2]
                        kb.op('pe', lambda e: e.matmul(pr[:], lhsT=pw[b][:, 0:128], rhs=Rm[rcur][b][:], start=True, stop=True),
                              reads=[pw[b], Rm[rcur][b]], writes=[pr])
                        kb.op('dve', lambda e: e.tensor_tensor(out=Rm[1 - rcur][b][:], in0=pr[:], in1=Rm[rcur][b][:], op=ALU.add),
                              reads=[pr, Rm[rcur][b]], writes=[Rm[1 - rcur][b]])
                    rcur = 1 - rcur
                    curM = [pw[b][:, 0:128] for b in range(NB)]
                    curN = [pw[b][:, 128:256] for b in range(NB)]
                    curMt = [pw[b] for b in range(NB)]
                    curNt = [pw[b] for b in range(NB)]
                for b in range(NB if stage >= 5 else 0):
                    blk = g0 + b
                    R5 = Rm[rcur][b]
                    kb.op('pe', lambda e: e.matmul(puw[:], lhsT=R5[:], rhs=rw[:, b, :], start=True, stop=True), reads=[R5, rw], writes=[puw])
                    kb.op('act', lambda e: e.activation(out=uw[b][:], in_=puw[:], func=AF.Copy), reads=[puw], writes=[uw[b]])
                    u_, w_ = uw[b][:, 0:128], uw[b][:, 128:256]
                    kb.op('pe', lambda e: e.matmul(pwa[:], lhsT=w_, rhs=ATm[b][:], start=True, stop=True), reads=[uw[b], ATm[b]], writes=[pwa])
                    kb.op('dve', lambda e: e.tensor_tensor(out=QpT[b][:], in0=TT[b][:, 3, :], in1=pwa[:], op=ALU.subtract),
                          reads=[TT[b], pwa], writes=[QpT[b]])
                    kb.op('pe', lambda e: e.matmul(pau[:], lhsT=ATm[b][:], rhs=u_, start=True, stop=True), reads=[ATm[b], uw[b]], writes=[pau])
                    kb.op('act', lambda e: e.activation(out=au[b][:], in_=pau[:], func=AF.Copy), reads=[pau], writes=[au[b]])
                    for c in range(2):
                        r0, r1 = c * 64, (c + 1) * 64
                        kb.op('pe', lambda e: e.matmul(pa[:], lhsT=uw[b][:, 128:256], rhs=kdm[:, b, c, :], start=True, stop=True),
                              reads=[uw[b], kdm], writes=[pa])
                        kb.op('dve', lambda e: e.scalar_tensor_tensor(out=AcT[2 * b + c][:], in0=ident[:], scalar=cd[:, c, blk:blk + 1], in1=pa[:],
                                                                      op0=ALU.mult, op1=ALU.subtract),
                              reads=[ident, cd, pa], writes=[AcT[2 * b + c]])
                for b in range(NB if stage >= 6 else 0):
                    for c in range(2):
                        r0, r1 = c * 64, (c + 1) * 64
                        Sc, Sn = S[scur], S[1 - scur]
                        kb.op('pe', lambda e: e.matmul(po2[c][:], lhsT=QpT[b][:], rhs=Sc[:], start=True, stop=True),
                              reads=[QpT[b], Sc], writes=[po2[c]])
                        kb.op('pe', lambda e: e.matmul(pss[:], lhsT=AcT[2 * b + c][:], rhs=Sc[:], start=True, stop=False),
                              reads=[AcT[2 * b + c], Sc], writes=[pss])
                        kb.op('pe', lambda e: e.matmul(pss[:], lhsT=kdm[:, b, c, :], rhs=uw[b][:, 0:128], start=False, stop=True),
                              reads=[kdm, uw[b]], writes=[pss])
                        kb.op('act', lambda e: e.activation(out=Sn[:], in_=pss[:], func=AF.Copy), reads=[pss], writes=[Sn])
                        scur = 1 - scur
                    kb.op('dve', lambda e: e.tensor_tensor(out=osb[b][0:64, :], in0=po2[0][0:64, :], in1=au[b][0:64, :], op=ALU.add), reads=[po2[0], au[b]], writes=[osb[b]])
                    kb.op('dve', lambda e: e.tensor_tensor(out=osb[b][64:128, :], in0=po2[1][64:128, :], in1=au[b][64:128, :], op=ALU.add), reads=[po2[1], au[b]], writes=[osb[b]])
                    kb.op('act', lambda e: e.activation(out=junk[:], in_=osb[b][:], func=AF.Square, accum_out=oss[b][:]),
                          reads=[osb[b]], writes=[junk, oss[b]])
                    kb.op('act', lambda e: e.activation(out=oss[b][:], in_=oss[b][:], func=AF.Sqrt, scale=1.0 / 128, bias=1e-6),
                          reads=[oss[b]], writes=[oss[b]])
                    kb.op('dve', lambda e: e.reciprocal(out=oss[b][:], in_=oss[b][:]), reads=[oss[b]], writes=[oss[b]])
                    kb.op('dve', lambda e: e.scalar_tensor_tensor(out=ofin[b][:], in0=osb[b][:], scalar=oss[b][:, 0:1], in1=zw[:, b, :],
                                                                  op0=ALU.mult, op1=ALU.mult), reads=[osb[b], oss[b], zw], writes=[ofin[b]])
                    blk = g0 + b
                    kb.dma(oout[hh, blk * 128:(blk + 1) * 128, :], ofin[b][:], ofin[b], False)
        kb.finish(ofin)
    return nc


NEGB = -30000.0
NIT = 20
SEQ = 16384


def build_C(NQB=32):
    NQ = NQB * 128
    nc = bass.Bass("TRN2", target_bir_lowering=False)
    iqT = nc.dram_tensor("iqT", [64, 8, NQ], F32, kind="ExternalInput").ap()
    iwin = nc.dram_tensor("iw", [128, NQB, 8], F32, kind="ExternalInput").ap()
    ikT = nc.dram_tensor("ikT", [64, SEQ], F32, kind="ExternalInput").ap()
    qTin = nc.dram_tensor("qT", [128, 8, NQ], BF16, kind="ExternalInput").ap()
    kTin = nc.dram_tensor("kT", [8, 128, SEQ], BF16, kind="ExternalInput").ap()
    vin = nc.dram_tensor("v", [8, 128, SEQ // 128, 128], BF16, kind="ExternalInput").ap()
    admin = nc.dram_tensor("admis", [128, 512], F32, kind="ExternalInput").ap()
    oout = nc.dram_tensor("o", [NQ, 1024], F32, kind="ExternalOutput").ap()
    with ExitStack() as st:
        kb = KB(nc, st)
        identf, _ = make_ident(kb)
        negI = kb.sb([128, 128], BF16, 'negI')
        kb.op('dve', lambda e: e.tensor_scalar(out=negI[:], in0=identf[:], scalar1=NEGB, scalar2=None, op0=ALU.mult),
              reads=[identf], writes=[negI])
        admis = kb.sb([128, 512], F32, 'admis_sb')
        kb.dma(admis[:], admin[:, :], admis, True)
        pow2 = kb.sb([128, NIT], F32, 'pow2')
        for k in range(NIT):
            kb.op('pool', lambda e: e.memset(pow2[:, k:k + 1], 2.0 ** -(k + 1)), writes=[pow2])
        iw = kb.sb([128, NQB, 8], F32, 'iw_sb')
        kb.dma(iw[:], iwin[:, :, :], iw, True)
        kb.op('dve', lambda e: e.tensor_scalar(out=iw[:], in0=iw[:], scalar1=(8 ** -0.5) * (64 ** -0.5), scalar2=None, op0=ALU.mult),
              reads=[iw], writes=[iw])
        score = kb.sb([128, SEQ], F32, 'score')
        mask = kb.sb([128, SEQ], BF16, 'mask')
        ikc = [kb.sb([64, 2048], F32, f'ikc{i}') for i in range(2)]
        kTc = [kb.sb([128, 2048], BF16, f'kTc{i}') for i in range(2)]
        Vc = [kb.sb([128, 16, 129], BF16, f'Vc{i}') for i in range(2)]
        for i in range(2):
            kb.op('pool', lambda e: e.memset(Vc[i][:], 1.0), writes=[Vc[i]])
        iq = kb.sb([64, 8, 128], F32, 'iq_sb')
        qT = kb.sb([128, 8, 128], BF16, 'qT_sb')
        rel = [kb.sb([128, 512], F32, f'rel{i}') for i in range(3)]
        PT = [kb.sb([128, 4, 128], BF16, f'PT{i}') for i in range(2)]
        obuf = [kb.sb([128, 8, 128], F32, f'obuf{i}') for i in range(2)]
        sm = {n: kb.sb([128, 1], F32, 'sm_' + n) for n in ('A', 'lo', 'rng', 'mid', 'cnt', 't', 'rec')}
        steps = kb.sb([128, NIT], F32, 'steps')
        pidx = [kb.ps([128, 512], F32, f'pidx{i}') for i in range(4)]
        pqk = [kb.ps([128, 512], F32, f'pqk{i}') for i in range(2)]
        pacc = kb.ps([128, 512], F32, 'pacc')
        nidx = 0
        nrel = 0
        nqk = 0
        nkv = 0
        nik = 0
        for k in range(NQB):
            L = 512 * (k + 1)
            kb.dma(iq[:], iqT[:, :, k * 128:(k + 1) * 128], iq, True)
            kb.dma(qT[:], qTin[:, :, k * 128:(k + 1) * 128], qT, True)
            for c0 in range(0, L, 2048):
                cw = min(2048, L - c0)
                ik = ikc[nik % 2]
                nik += 1
                kb.dma(ik[:, 0:cw], ikT[:, c0:c0 + cw], ik, True)
                for s0 in range(0, cw, 512):
                    for h in range(8):
                        pi = pidx[nidx % 4]
                        nidx += 1
                        kb.op('pe', lambda e: e.matmul(pi[:], lhsT=iq[:, h, :], rhs=ik[:, s0:s0 + 512], start=True, stop=True),
                              reads=[iq, ik], writes=[pi])
                        r = rel[nrel % 3]
                        nrel += 1
                        kb.op('act', lambda e: e.activation(out=r[:], in_=pi[:], func=AF.Relu), reads=[pi], writes=[r])
                        dst = score[:, c0 + s0:c0 + s0 + 512]
                        if h == 0:
                            kb.op('dve', lambda e: e.tensor_scalar(out=dst, in0=r[:], scalar1=iw[:, k, 0:1], scalar2=None, op0=ALU.mult),
                                  reads=[r, iw], writes=[score])
                        else:
                            kb.op('dve', lambda e: e.scalar_tensor_tensor(out=dst, in0=r[:], scalar=iw[:, k, h:h + 1], in1=dst,
                                                                          op0=ALU.mult, op1=ALU.add), reads=[r, iw, score], writes=[score])
            A, lo, rng, mid, cnt, tt, rec = (sm[n] for n in ('A', 'lo', 'rng', 'mid', 'cnt', 't', 'rec'))
            kb.op('dve', lambda e: e.tensor_reduce(out=A[:], in_=score[:, 0:L], axis=AX.X, op=ALU.max, apply_absolute_value=True),
                  reads=[score], writes=[A])
            kb.op('dve', lambda e: e.tensor_scalar(out=lo[:], in0=A[:], scalar1=-1.0, scalar2=-1.0, op0=ALU.mult, op1=ALU.add),
                  reads=[A], writes=[lo])
            kb.op('dve', lambda e: e.tensor_scalar(out=rng[:], in0=A[:], scalar1=2.0, scalar2=2.0, op0=ALU.mult, op1=ALU.add),
                  reads=[A], writes=[rng])
            kb.op('dve', lambda e: e.tensor_scalar(out=steps[:], in0=pow2[:], scalar1=rng[:, 0:1], scalar2=None, op0=ALU.mult),
                  reads=[pow2, rng], writes=[steps])
            kb.op('pool', lambda e: e.tensor_tensor(out=score[:, L - 512:L], in0=score[:, L - 512:L], in1=admis[:], op=ALU.add),
                  reads=[score, admis], writes=[score])
            for it in range(NIT):
                kb.op('dve', lambda e: e.tensor_tensor(out=mid[:], in0=lo[:], in1=steps[:, it:it + 1], op=ALU.add),
                      reads=[lo, steps], writes=[mid])
                kb.op('dve', lambda e: e.tensor_scalar(out=mask[:, 0:L], in0=score[:, 0:L], scalar1=mid[:, 0:1], scalar2=None,
                                                       op0=ALU.is_ge, op1=ALU.add, accum_out=cnt[:]),
                      reads=[score, mid], writes=[mask, cnt])
                kb.op('dve', lambda e: e.tensor_scalar(out=tt[:], in0=cnt[:], scalar1=255.5, scalar2=None, op0=ALU.is_ge),
                      reads=[cnt], writes=[tt])
                kb.op('dve', lambda e: e.tensor_tensor(out=tt[:], in0=tt[:], in1=steps[:, it:it + 1], op=ALU.mult),
                      reads=[tt, steps], writes=[tt])
                kb.op('dve', lambda e: e.tensor_tensor(out=lo[:], in0=lo[:], in1=tt[:], op=ALU.add), reads=[lo, tt], writes=[lo])
            kb.op('dve', lambda e: e.tensor_scalar(out=mask[:, 0:L], in0=score[:, 0:L], scalar1=lo[:, 0:1], scalar2=None, op0=ALU.is_lt),
                  reads=[score, lo], writes=[mask])
            ob = obuf[k % 2]
            ntile = L // 128
            for h in range(8):
                for c0 in range(0, L, 2048):
                    cw = min(2048, L - c0)
                    kt, vc = kTc[nkv % 2], Vc[nkv % 2]
                    nkv += 1
                    kb.dma(kt[:, 0:cw], kTin[h, :, c0:c0 + cw], kt, True)
                    kb.dma(vc[:, 0:cw // 128, 0:128], vin[h, :, c0 // 128:(c0 + cw) // 128, :], vc, True)
                    for g0 in range(0, cw, 512):
                        pq = pqk[nqk % 2]
                        pt = PT[nqk % 2]
                        nqk += 1
                        for j in range(4):
                            s = c0 + g0 + j * 128
                            kb.op('pe', lambda e: e.matmul(pq[:, j * 128:(j + 1) * 128], lhsT=kt[:, g0 + j * 128:g0 + (j + 1) * 128], rhs=qT[:, h, :],
                                                           start=True, stop=False), reads=[kt, qT], writes=[pq])
                            kb.op('pe', lambda e: e.matmul(pq[:, j * 128:(j + 1) * 128], lhsT=mask[:, s:s + 128], rhs=negI[:],
                                                           start=False, stop=True), reads=[mask, negI], writes=[pq])
                        kb.op('act', lambda e: e.activation(out=pt[:].rearrange("p a b -> p (a b)"), in_=pq[:], func=AF.Exp, scale=128.0 ** -0.5),
                              reads=[pq], writes=[pt])
                        for j in range(4):
                            ti = (c0 + g0) // 128 + j
                            kb.op('pe', lambda e: e.matmul(pacc[:, 0:129], lhsT=pt[:, j, :], rhs=vc[:, (g0 // 128) + j, :],
                                                           start=(ti == 0), stop=(ti == ntile - 1)), reads=[pt, vc], writes=[pacc])
                kb.op('dve', lambda e: e.reciprocal(out=rec[:], in_=pacc[:, 128:129]), reads=[pacc], writes=[rec])
                kb.op('dve', lambda e: e.tensor_scalar(out=ob[:, h, :], in0=pacc[:, 0:128], scalar1=rec[:, 0:1], scalar2=None, op0=ALU.mult),
                      reads=[pacc, rec], writes=[ob])
            kb.dma(oout[k * 128:(k + 1) * 128, :], ob[:].rearrange("p a b -> p (a b)"), ob, False)
        kb.finish(obuf)
    return nc


def admis_mask(j):
    m = np.zeros((128, 512), np.float32)
    o = np.arange(512)[None, :]
    lim = np.where(np.arange(128)[:, None] < 64, 128 * j + 64, 128 * j + 128)
    m[o >= lim] = NEGB
    return m


D = 1024
DFF = 2816


def build_D(NT=4096, final=False):
    nt = NT // 128
    nc = bass.Bass("TRN2", target_bir_lowering=False)
    def din(name, shape, dt=F32):
        return nc.dram_tensor(name, shape, dt, kind="ExternalInput").ap()
    x = din("x", [NT, D]); xq = din("xq", [NT, D]); gl = din("gl", [NT, 3 * D]); odn = din("odn", [NT, D]); osa = din("osa", [NT, D])
    mem = din("mem", [256, D]); gmem = din("gmem", [128, 8]); wkv = din("wkv", [D, 2 * D])
    wbr = din("wbr", [3, D, D]); wout = din("wout", [D, D]); bg = din("bg", [128, 3 * D])
    gffn = din("gffn", [128, 8]); wfi = din("wfi", [D, 2 * DFF]); wfo = din("wfo", [DFF, D]); gfin = din("gfin", [128, D])
    y = nc.dram_tensor("y", [NT, D], F32, kind="ExternalOutput").ap()
    with ExitStack() as st:
        kb = KB(nc, st)
        ident, ones = make_ident(kb)
        ones_b = kb.sb([128, 1], BF16, 'ones_b')
        kb.op('pool', lambda e: e.memset(ones_b[:], 1.0), writes=[ones_b])
        banks = [kb.ps([128, 512], F32, f'bank{i}') for i in range(8)]
        wf = [kb.sb([128, 22, 256], F32, f'wf{i}') for i in range(2)]
        wb = [kb.sb([128, 22, 256], BF16, f'wb{i}') for i in range(2)]
        cnt = {'w': 0, 'tp': 0, 'mm': 0}
        ones_g = kb.sb([128, 22], F32, 'ones_g')
        kb.op('pool', lambda e: e.memset(ones_g[:], 1.0), writes=[ones_g])

        def small_in(ap_, shape, name):
            t = kb.sb(shape, F32, name)
            kb.dma(t[:], ap_, t, True)
            return t
        gmem_sb = small_in(gmem[:, :], [128, 8], 'gmem_sb')
        gffn_sb = small_in(gffn[:, :], [128, 8], 'gffn_sb')
        bg_sb = small_in(bg[:, :], [128, 3 * D], 'bg_sb')
        gfin_sb = small_in(gfin[:, :], [128, D], 'gfin_sb') if final else None

        def transp(src, KC, dst):
            for c0 in range(0, KC, 4):
                n = min(4, KC - c0)
                pt = banks[cnt['tp'] % 2]
                cnt['tp'] += 1
                for j in range(n):
                    kb.op('pe', lambda e: e.transpose(out=pt[:, j * 128:(j + 1) * 128], in_=src[:, (c0 + j) * 128:(c0 + j + 1) * 128], identity=ident[:]),
                          reads=[src, ident], writes=[pt])
                kb.op('act', lambda e: e.activation(out=dst[:, c0:c0 + n, :], in_=pt[:, 0:n * 128].rearrange("p (a b) -> p a b", a=n), func=AF.Copy),
                      reads=[pt], writes=[dst])

        def lin(hT, KC, W, c0, cw, gcol=None):
            i = cnt['w'] % 2
            cnt['w'] += 1
            kb.dma(wf[i][:, 0:KC, 0:cw], W[:, c0:c0 + cw].rearrange("(kc k) n -> k kc n", k=128), wf[i], True)
            for kc in range(KC):
                sc = gcol[:, kc:kc + 1] if gcol is not None else ones_g[:, kc:kc + 1]
                kb.op('pool', lambda e: e.tensor_scalar(out=wb[i][:, kc, 0:cw], in0=wf[i][:, kc, 0:cw], scalar1=sc, scalar2=None, op0=ALU.mult),
                      reads=[wf[i], gcol if gcol is not None else ones_g], writes=[wb[i]])
            pm = banks[2 + cnt['mm'] % 4]
            cnt['mm'] += 1
            for kc in range(KC):
                kb.op('pe', lambda e: e.matmul(pm[:, 0:cw], lhsT=hT[:, kc, :], rhs=wb[i][:, kc, 0:cw], start=(kc == 0), stop=(kc == KC - 1)),
                      reads=[hT, wb[i]], writes=[pm])
            return pm

        def rms_scale(src, dst, ss, width):
            junk_ = junk
            kb.op('act', lambda e: e.activation(out=junk_[:, 0:width], in_=src[:, 0:width], func=AF.Square, accum_out=ss[:]), reads=[src], writes=[junk_, ss])
            kb.op('act', lambda e: e.activation(out=ss[:], in_=ss[:], func=AF.Sqrt, scale=1.0 / width, bias=1e-6), reads=[ss], writes=[ss])
            kb.op('dve', lambda e: e.reciprocal(out=ss[:], in_=ss[:]), reads=[ss], writes=[ss])
            kb.op('dve', lambda e: e.tensor_scalar(out=dst[:, 0:width], in0=src[:, 0:width], scalar1=ss[:, 0:1], scalar2=None, op0=ALU.mult),
                  reads=[src, ss], writes=[dst])

        junk = kb.sb([128, DFF], F32, 'junk')
        ss = kb.sb([128, 1], F32, 'ss')
        hT = kb.sb([128, 22, 128], BF16, 'hT')
        mkT = kb.sb([128, 8, 256], BF16, 'mkT')
        mv = kb.sb([128, 2, 1024], BF16, 'mv')
        mt = kb.sb([128, D], F32, 'mt')
        mn = kb.sb([128, D], F32, 'mn')
        mkrow = kb.sb([128, D], F32, 'mkrow')
        for mc in range(2):
            kb.dma(mt[:], mem[mc * 128:(mc + 1) * 128, :], mt, True)
            rms_scale(mt, mn, ss, D)
            transp(mn, 8, hT)
            for c0 in range(0, 2 * D, 256):
                pm = lin(hT, 8, wkv, c0, 256, gmem_sb)
                if c0 < D:
                    kb.op('dve', lambda e: e.tensor_copy(out=mkrow[:, c0:c0 + 256], in_=pm[:, 0:256]), reads=[pm], writes=[mkrow])
                else:
                    kb.op('dve', lambda e: e.tensor_copy(out=mv[:, mc, c0 - D:c0 - D + 256], in_=pm[:, 0:256]), reads=[pm], writes=[mv])
            for c0 in range(0, 8, 4):
                pt = banks[cnt['tp'] % 2]
                cnt['tp'] += 1
                for j in range(4):
                    kb.op('pe', lambda e: e.transpose(out=pt[:, j * 128:(j + 1) * 128], in_=mkrow[:, (c0 + j) * 128:(c0 + j + 1) * 128], identity=ident[:]),
                          reads=[mkrow, ident], writes=[pt])
                kb.op('act', lambda e: e.activation(out=mkT[:, c0:c0 + 4, mc * 128:(mc + 1) * 128], in_=pt[:].rearrange("p (a b) -> p a b", a=4), func=AF.Copy),
                      reads=[pt], writes=[mkT])

        def tile_in(name, w_):
            return kb.sb([128, w_], F32, name)
        xt = tile_in('xt', D); xqt = tile_in('xqt', D); glt = tile_in('glt', 3 * D); odt = tile_in('odt', D); ost = tile_in('ost', D)
        oxa = tile_in('oxa', D); merged = tile_in('merged', D); tmpm = tile_in('tmpm', 256); x1 = tile_in('x1', D); xn = tile_in('xn', D)
        act = tile_in('act', DFF); sil = tile_in('sil', 256); x2 = [tile_in(f'x2_{i}', D) for i in range(2)]
        PTm = kb.sb([128, 8, 128], BF16, 'PTm')
        rsum = kb.sb([128, 4], F32, 'rsum')
        for i in range(nt):
            r = slice(i * 128, (i + 1) * 128)
            for t_, src in ((xt, x), (xqt, xq), (glt, gl), (odt, odn), (ost, osa)):
                kb.dma(t_[:], src[r, :], t_, True)
            transp(xqt, 8, hT)
            for hp in range(2):
                pl = banks[6 + hp]
                for hh in range(2):
                    h = hp * 2 + hh
                    for mc in range(2):
                        col = (hh * 2 + mc) * 128
                        for dc in range(2):
                            kb.op('pe', lambda e: e.matmul(pl[:, col:col + 128], lhsT=mkT[:, h * 2 + dc, mc * 128:(mc + 1) * 128], rhs=hT[:, h * 2 + dc, :],
                                                           start=(dc == 0), stop=(dc == 1)), reads=[mkT, hT], writes=[pl])
                kb.op('act', lambda e: e.activation(out=PTm[:, hp * 4:(hp + 1) * 4, :], in_=pl[:].rearrange("p (a b) -> p a b", a=4), func=AF.Exp, scale=256.0 ** -0.5),
                      reads=[pl], writes=[PTm])
            pv = [banks[2], banks[3]]
            psm = banks[4]
            for h in range(4):
                for mc in range(2):
                    kb.op('pe', lambda e: e.matmul(pv[h // 2][:, (h % 2) * 256:(h % 2) * 256 + 256], lhsT=PTm[:, h * 2 + mc, :], rhs=mv[:, mc, h * 256:(h + 1) * 256],
                                                   start=(mc == 0), stop=(mc == 1)), reads=[PTm, mv], writes=[pv[h // 2]])
                for mc in range(2):
                    kb.op('pe', lambda e: e.matmul(psm[:, h:h + 1], lhsT=PTm[:, h * 2 + mc, :], rhs=ones_b[:], start=(mc == 0), stop=(mc == 1)),
                          reads=[PTm, ones_b], writes=[psm])
            kb.op('dve', lambda e: e.reciprocal(out=rsum[:], in_=psm[:, 0:4]), reads=[psm], writes=[rsum])
            for h in range(4):
                kb.op('dve', lambda e: e.tensor_scalar(out=oxa[:, h * 256:(h + 1) * 256], in0=pv[h // 2][:, (h % 2) * 256:(h % 2) * 256 + 256],
                                                       scalar1=rsum[:, h:h + 1], scalar2=None, op0=ALU.mult), reads=[pv[h // 2], rsum], writes=[oxa])
            kb.op('dve', lambda e: e.tensor_tensor(out=glt[:], in0=glt[:], in1=bg_sb[:], op=ALU.add), reads=[glt, bg_sb], writes=[glt])
            kb.op('act', lambda e: e.activation(out=glt[:], in_=glt[:], func=AF.Sigmoid), reads=[glt], writes=[glt])
            for b, src in enumerate((odt, ost, oxa)):
                transp(src, 8, hT)
                for c0 in range(0, D, 256):
                    pm = lin(hT, 8, wbr[b], c0, 256)
                    if b == 0:
                        kb.op('dve', lambda e: e.tensor_tensor(out=merged[:, c0:c0 + 256], in0=pm[:, 0:256], in1=glt[:, b * D + c0:b * D + c0 + 256], op=ALU.mult),
                              reads=[pm, glt], writes=[merged])
                    else:
                        kb.op('dve', lambda e: e.tensor_tensor(out=tmpm[:], in0=pm[:, 0:256], in1=glt[:, b * D + c0:b * D + c0 + 256], op=ALU.mult),
                              reads=[pm, glt], writes=[tmpm])
                        kb.op('pool', lambda e: e.tensor_tensor(out=merged[:, c0:c0 + 256], in0=merged[:, c0:c0 + 256], in1=tmpm[:], op=ALU.add),
                              reads=[merged, tmpm], writes=[merged])
            transp(merged, 8, hT)
            for c0 in range(0, D, 256):
                pm = lin(hT, 8, wout, c0, 256)
                kb.op('dve', lambda e: e.tensor_tensor(out=x1[:, c0:c0 + 256], in0=pm[:, 0:256], in1=xt[:, c0:c0 + 256], op=ALU.add), reads=[pm, xt], writes=[x1])
            rms_scale(x1, xn, ss, D)
            transp(xn, 8, hT)
            for c0 in range(0, DFF, 256):
                pg_ = lin(hT, 8, wfi, c0, 256, gffn_sb)
                kb.op('act', lambda e: e.activation(out=sil[:], in_=pg_[:, 0:256], func=AF.Silu), reads=[pg_], writes=[sil])
                pu_ = lin(hT, 8, wfi, DFF + c0, 256, gffn_sb)
                kb.op('dve', lambda e: e.tensor_tensor(out=act[:, c0:c0 + 256], in0=pu_[:, 0:256], in1=sil[:], op=ALU.mult), reads=[pu_, sil], writes=[act])
            transp(act, 22, hT)
            xo = x2[i % 2]
            for c0 in range(0, D, 256):
                pm = lin(hT, 22, wfo, c0, 256)
                kb.op('dve', lambda e: e.tensor_tensor(out=xo[:, c0:c0 + 256], in0=pm[:, 0:256], in1=x1[:, c0:c0 + 256], op=ALU.add), reads=[pm, x1], writes=[xo])
            if final:
                rms_scale(xo, xn, ss, D)
                kb.op('dve', lambda e: e.tensor_tensor(out=xo[:], in0=xn[:], in1=gfin_sb[:], op=ALU.mult), reads=[xn, gfin_sb], writes=[xo])
            kb.dma(y[r, :], xo[:], xo, False)
        kb.finish(x2)
    return nc


def _col8(v):
    return np.ascontiguousarray(np.asarray(v, np.float32).reshape(8, 128).T)


def kernel(x, mem, positions, norm_mix, norm_mem, w_in, b_gate, conv_w, a_log, dt_bias,
           dn_norm, w_mem_kv, w_branch, w_out, norm_ffn, w_ffn_in, w_ffn_out, norm_final):
    f32 = np.float32
    x = np.asarray(x, f32)
    mem = np.asarray(mem, f32)
    positions = np.asarray(positions, np.int32)
    ncores = 8
    cores = list(range(ncores))
    S = 16384
    NT = 4096
    isa, iix = host_consts()
    ncA = build_A(NT)
    ncB = build_B(NH=2)
    ncC = build_C(32)
    xcur = x
    for l in range(2):
        maps = []
        for c in cores:
            b, j = c // 4, c % 4
            r = slice(j * NT, (j + 1) * NT)
            maps.append(dict(x=np.ascontiguousarray(xcur[b, r]),
                             pos=np.ascontiguousarray(positions[b, r].reshape(NT // 128, 128).T),
                             gcol=_col8(norm_mix[l]), w=np.asarray(w_in[l], f32), invf_sa=isa, invf_ix=iix))
        res = run_bass_kernel_spmd(ncA, maps, core_ids=cores)
        P = np.stack([np.concatenate([res.results[b * 4 + j]["P"] for j in range(4)], 0) for b in range(2)])
        Pb = np.stack([np.concatenate([res.results[b * 4 + j]["Pb"] for j in range(4)], 0) for b in range(2)])
        del res
        cw_l = np.asarray(conv_w[l], f32)
        maps = []
        for c in cores:
            b, hp_ = c // 4, c % 4
            xp = np.zeros((2, S + 3, 384), f32)
            z = np.empty((2, S, 128), f32)
            ba = np.empty((2, 128, 2, S // 128), f32)
            wc = np.empty((2, 128, 4, 384), f32)
            hp = np.empty((2, 128, 2), f32)
            for i in range(2):
                h = hp_ * 2 + i
                for t in range(3):
                    xp[i, 3:, t * 128:(t + 1) * 128] = P[b, :, t * 1024 + h * 128:t * 1024 + (h + 1) * 128]
                    wc[i, :, :, t * 128:(t + 1) * 128] = cw_l[None, :, t * 1024 + h * 128:t * 1024 + (h + 1) * 128]
                z[i] = P[b, :, 3072 + h * 128:3072 + (h + 1) * 128]
                ba[i, :, 0, :] = P[b, :, 4096 + h].reshape(S // 128, 128).T
                ba[i, :, 1, :] = P[b, :, 4104 + h].reshape(S // 128, 128).T
                hp[i, :, 0] = a_log[l][h]
                hp[i, :, 1] = dt_bias[l][h]
            maps.append(dict(xp=xp, z=z, ba=ba, wc=wc, hp=hp,
                             dnw=np.ascontiguousarray(np.tile(np.asarray(dn_norm[l], f32)[None], (128, 1)))))
        res = run_bass_kernel_spmd(ncB, maps, core_ids=cores)
        odn = np.empty((2, S, 1024), f32)
        for c in cores:
            b, hp_ = c // 4, c % 4
            for i in range(2):
                h = hp_ * 2 + i
                odn[b, :, h * 128:(h + 1) * 128] = res.results[c]["o"][i]
        del res, maps
        maps = []
        toks = []
        for b in range(2):
            kT = np.ascontiguousarray(Pb[b][:, 1024:2048].reshape(S, 8, 128).transpose(1, 2, 0))
            vv = np.ascontiguousarray(Pb[b][:, 2048:3072].reshape(128, 128, 8, 128).transpose(2, 1, 0, 3))
            ikT = np.ascontiguousarray(P[b][:, 7696:7760].T)
            for j in range(4):
                tk = np.concatenate([np.arange((4 * k + j) * 128, (4 * k + j + 1) * 128) for k in range(32)])
                toks.append(tk)
                maps.append(dict(iqT=np.ascontiguousarray(P[b][tk, 7184:7696].reshape(-1, 8, 64).transpose(2, 1, 0)),
                                 iw=np.ascontiguousarray(P[b][tk, 7760:7768].reshape(32, 128, 8).transpose(1, 0, 2)),
                                 ikT=ikT,
                                 qT=np.ascontiguousarray(Pb[b][tk, 0:1024].reshape(-1, 8, 128).transpose(2, 1, 0)),
                                 kT=kT, v=vv, admis=admis_mask(j)))
        res = run_bass_kernel_spmd(ncC, maps, core_ids=cores)
        osa = np.empty((2, S, 1024), f32)
        for c in cores:
            osa[c // 4][toks[c]] = res.results[c]["o"]
        del res, maps
        ncD = build_D(NT, final=(l == 1))
        maps = []
        for c in cores:
            b, j = c // 4, c % 4
            r = slice(j * NT, (j + 1) * NT)
            maps.append(dict(x=np.ascontiguousarray(xcur[b, r]), xq=np.ascontiguousarray(P[b, r, 7768:8792]),
                             gl=np.ascontiguousarray(P[b, r, 8792:11864]), odn=np.ascontiguousarray(odn[b, r]),
                             osa=np.ascontiguousarray(osa[b, r]), mem=np.ascontiguousarray(mem[b]), gmem=_col8(norm_mem[l]),
                             wkv=np.asarray(w_mem_kv[l], f32), wbr=np.asarray(w_branch[l], f32), wout=np.asarray(w_out[l], f32),
                             bg=np.ascontiguousarray(np.tile(np.asarray(b_gate[l], f32).reshape(1, -1), (128, 1))),
                             gffn=_col8(norm_ffn[l]), wfi=np.asarray(w_ffn_in[l], f32), wfo=np.asarray(w_ffn_out[l], f32),
                             gfin=np.ascontiguousarray(np.tile(np.asarray(norm_final, f32)[None], (128, 1)))))
        res = run_bass_kernel_spmd(ncD, maps, core_ids=cores)
        xcur = np.stack([np.concatenate([res.results[b * 4 + j]["y"] for j in range(4)], 0) for b in range(2)])
        del res, maps, P, Pb, odn, osa
    return xcur.astype(np.float32)
```

```python
import math
import ml_dtypes
import numpy as np
import concourse.bass as bass
import concourse.mybir as mybir
from concourse.bass_utils import run_bass_kernel_spmd
from contextlib import ExitStack

F32 = mybir.dt.float32
BF16 = mybir.dt.bfloat16
I32 = mybir.dt.int32
AF = mybir.ActivationFunctionType
ALU = mybir.AluOpType
AX = mybir.AxisListType

SAME_ENGINE_WAIT = True


class T:
    def __init__(self, kb, t, name):
        self.kb = kb
        self.t = t
        self.name = name
        self.writer = None
        self.readers = []
        self.dsem = None
        self.dcnt = 0
        self.dma_pending_w = False
        self.dma_pending_r = False
        self.root = self

    def __getitem__(self, idx):
        return self.t[idx]


class KB:
    def __init__(self, nc, stack):
        self.nc = nc
        self.stack = stack
        self.engs = {'pe': nc.tensor, 'dve': nc.vector, 'act': nc.scalar, 'pool': nc.gpsimd, 'sp': nc.sync}
        self.sem = {n: stack.enter_context(nc.semaphore('prog_' + n)) for n in self.engs}
        self.cnt = {n: 0 for n in self.engs}
        self.waited = {n: {} for n in self.engs}
        self.ntile = 0

    def sb(self, shape, dt=F32, name=None):
        self.ntile += 1
        name = name or f"t{self.ntile}"
        t = self.stack.enter_context(self.nc.sbuf_tensor(name, list(shape), dt))
        return T(self, t, name)

    def ps(self, shape, dt=F32, name=None):
        self.ntile += 1
        name = name or f"p{self.ntile}"
        t = self.stack.enter_context(self.nc.psum_tensor(name, list(shape), dt))
        return T(self, t, name)

    def view(self, tile, lo, hi, name):
        v = T(self, tile.t[:, lo:hi], name)
        v.root = tile.root
        return v

    def _dsem(self, tile):
        if tile.dsem is None:
            tile.dsem = self.stack.enter_context(self.nc.semaphore('d_' + tile.name))
        return tile.dsem

    def _wait(self, eng, key, sem, val):
        w = self.waited[eng]
        if w.get(key, 0) < val:
            self.engs[eng].wait_ge(sem, val)
            w[key] = val

    def _deps(self, eng, reads, writes):
        deps = {}
        reads = [t.root for t in reads]
        writes = [t.root for t in writes]

        def add(e, i):
            if e == eng and (eng == 'pe' or not SAME_ENGINE_WAIT):
                return
            deps[e] = max(deps.get(e, 0), i)

        for t in reads:
            if t.writer:
                add(*t.writer)
            if t.dsem is not None and t.dma_pending_w:
                self._wait(eng, ('d', t.name), t.dsem, 16 * t.dcnt)
        for t in writes:
            if t.writer:
                add(*t.writer)
            for r in t.readers:
                add(*r)
            if t.dsem is not None and (t.dma_pending_w or t.dma_pending_r):
                self._wait(eng, ('d', t.name), t.dsem, 16 * t.dcnt)
        for e, i in deps.items():
            self._wait(eng, e, self.sem[e], i)

    def op(self, eng, fn, reads=(), writes=()):
        self._deps(eng, reads, writes)
        inst = fn(self.engs[eng])
        inst.then_inc(self.sem[eng], 1)
        self.cnt[eng] += 1
        me = (eng, self.cnt[eng])
        reads = [t.root for t in reads]
        writes = [t.root for t in writes]
        for t in reads:
            t.readers.append(me)
            if len(t.readers) > 64:
                t.readers = self._prune(t.readers)
        for t in writes:
            t.writer = me
            t.readers = []
            t.dma_pending_w = False
            t.dma_pending_r = False
        return inst

    @staticmethod
    def _prune(rs):
        best = {}
        for e, i in rs:
            best[e] = max(best.get(e, 0), i)
        return list(best.items())

    def dma(self, out, in_, tile, is_load, q='sp', **kw):
        if is_load:
            self._deps(q, (), (tile,))
        else:
            self._deps(q, (tile,), ())
        sem = self._dsem(tile)
        inst = self.engs[q].dma_start(out=out, in_=in_, **kw)
        inst.then_inc(sem, 16)
        tile.dcnt += 1
        if is_load:
            tile.writer = None
            tile.readers = []
            tile.dma_pending_w = True
        else:
            tile.dma_pending_r = True
        return inst

    def finish(self, tiles, eng='sp'):
        for t in tiles:
            if t.dsem is not None:
                self.engs[eng].wait_ge(t.dsem, 16 * t.dcnt)


D = 1024
N_IN = 11864
SEGS = [('dq', 0, 1024), ('dk', 1024, 1024), ('dv', 2048, 1024), ('dz', 3072, 1024), ('dba', 4096, 16),
        ('sq', 4112, 1024), ('sk', 5136, 1024), ('sv', 6160, 1024), ('iq', 7184, 512), ('ikw', 7696, 72),
        ('xq', 7768, 1024), ('gl', 8792, 3072)]
TWO_PI = 2.0 * math.pi


def rope_tables(kb, posf, invf, nt, half, name):
    ang = kb.sb([128, nt, half], F32, name + '_ang')
    kb.op('dve', lambda e: e.tensor_tensor(out=ang[:], in0=invf[:].unsqueeze(1).to_broadcast([128, nt, half]),
                                           in1=posf[:].unsqueeze(2).to_broadcast([128, nt, half]), op=ALU.mult),
          reads=[posf, invf], writes=[ang])
    r = kb.sb([128, nt, half], F32, name + '_r')
    kb.op('dve', lambda e: e.tensor_scalar(out=r[:], in0=ang[:], scalar1=1.0 / TWO_PI, scalar2=None, op0=ALU.mult),
          reads=[ang], writes=[r])
    ni = kb.sb([128, nt, half], I32, name + '_ni')
    kb.op('dve', lambda e: e.tensor_copy(out=ni[:], in_=r[:]), reads=[r], writes=[ni])
    nf = kb.sb([128, nt, half], F32, name + '_nf')
    kb.op('dve', lambda e: e.tensor_copy(out=nf[:], in_=ni[:]), reads=[ni], writes=[nf])
    fr = kb.sb([128, nt, half], F32, name + '_fr')
    kb.op('dve', lambda e: e.tensor_tensor(out=fr[:], in0=r[:], in1=nf[:], op=ALU.subtract), reads=[r, nf], writes=[fr])
    def wrapped(shift, nm2):
        f = kb.sb([128, nt, half], F32, name + nm2 + '_f')
        t = kb.sb([128, nt, half], F32, name + nm2 + '_t')
        kb.op('dve', lambda e: e.tensor_scalar(out=f[:], in0=fr[:], scalar1=shift, scalar2=None, op0=ALU.add),
              reads=[fr], writes=[f])
        for _ in range(2):
            kb.op('dve', lambda e: e.tensor_scalar(out=t[:], in0=f[:], scalar1=0.5, scalar2=-1.0, op0=ALU.is_gt, op1=ALU.mult),
                  reads=[f], writes=[t])
            kb.op('dve', lambda e: e.tensor_tensor(out=f[:], in0=f[:], in1=t[:], op=ALU.add), reads=[f, t], writes=[f])
        kb.op('dve', lambda e: e.tensor_scalar(out=t[:], in0=f[:], scalar1=-0.5, scalar2=None, op0=ALU.is_lt),
              reads=[f], writes=[t])
        kb.op('dve', lambda e: e.tensor_tensor(out=f[:], in0=f[:], in1=t[:], op=ALU.add), reads=[f, t], writes=[f])
        kb.op('dve', lambda e: e.tensor_scalar(out=f[:], in0=f[:], scalar1=TWO_PI, scalar2=3.14159, op0=ALU.mult, op1=ALU.min),
              reads=[f], writes=[f])
        kb.op('dve', lambda e: e.tensor_scalar(out=f[:], in0=f[:], scalar1=-3.14159, scalar2=None, op0=ALU.max),
              reads=[f], writes=[f])
        o = kb.sb([128, nt, half], F32, name + nm2)
        kb.op('act', lambda e: e.activation(out=o[:], in_=f[:], func=AF.Sin), reads=[f], writes=[o])
        return o
    sin = wrapped(0.0, '_sin')
    cos = wrapped(0.25, '_cos')
    return cos, sin


def make_ident(kb, dt=F32, name='ident'):
    ones = kb.sb([128, 128], F32, name + '_ones')
    ident = kb.sb([128, 128], dt, name)
    kb.op('pool', lambda e: e.memset(ones[:], 1.0), writes=[ones])
    kb.op('pool', lambda e: e.affine_select(out=ident[:], in_=ones[:], pattern=[[-1, 128]], compare_op=ALU.is_equal,
                                            fill=0.0, base=0, channel_multiplier=1), reads=[ones], writes=[ident])
    return ident, ones


def build_A(NT=4096, with_rope=True):
    nt = NT // 128
    nc = bass.Bass("TRN2", target_bir_lowering=False)
    x = nc.dram_tensor("x", [NT, D], F32, kind="ExternalInput").ap()
    pos = nc.dram_tensor("pos", [128, nt], I32, kind="ExternalInput").ap()
    gcol = nc.dram_tensor("gcol", [128, 8], F32, kind="ExternalInput").ap()
    w = nc.dram_tensor("w", [D, N_IN], F32, kind="ExternalInput").ap()
    invf_sa = nc.dram_tensor("invf_sa", [128, 16], F32, kind="ExternalInput").ap()
    invf_ix = nc.dram_tensor("invf_ix", [128, 8], F32, kind="ExternalInput").ap()
    P = nc.dram_tensor("P", [NT, N_IN], F32, kind="ExternalOutput").ap()
    Pb = nc.dram_tensor("Pb", [NT, 3072], BF16, kind="ExternalOutput").ap()
    with ExitStack() as st:
        kb = KB(nc, st)
        ident, _ = make_ident(kb)
        g_sb = kb.sb([128, 8], F32, 'g_sb')
        kb.dma(g_sb[:], gcol[:, :], g_sb, True)
        posi = kb.sb([128, nt], I32, 'posi')
        kb.dma(posi[:], pos[:, :], posi, True)
        posf = kb.sb([128, nt], F32, 'posf')
        kb.op('dve', lambda e: e.tensor_copy(out=posf[:], in_=posi[:]), reads=[posi], writes=[posf])
        ifs = kb.sb([128, 16], F32, 'ifs')
        ifx = kb.sb([128, 8], F32, 'ifx')
        kb.dma(ifs[:], invf_sa[:, :], ifs, True)
        kb.dma(ifx[:], invf_ix[:, :], ifx, True)
        cos_sa, sin_sa = rope_tables(kb, posf, ifs, nt, 16, 'rsa')
        cos_ix, sin_ix = rope_tables(kb, posf, ifx, nt, 8, 'rix')

        hT = kb.sb([128, 8, NT], BF16, 'hT')
        xts = [kb.sb([128, D], F32, f'xt{i}') for i in range(2)]
        xns = [kb.sb([128, D], F32, f'xn{i}') for i in range(2)]
        junk = kb.sb([128, D], F32, 'junk')
        sss = [kb.sb([128, 1], F32, f'ss{i}') for i in range(2)]
        rss = [kb.sb([128, 1], F32, f'rs{i}') for i in range(2)]
        ptr = [kb.ps([128, 512], F32, f'ptr{i}') for i in range(2)]
        pmm = [kb.ps([128, 512], F32, f'pmm{i}') for i in range(4)]
        for i in range(nt):
            xt, xn, ss, rs = xts[i % 2], xns[i % 2], sss[i % 2], rss[i % 2]
            kb.dma(xt[:], x[i * 128:(i + 1) * 128, :], xt, True)
            kb.op('act', lambda e: e.activation(out=junk[:], in_=xt[:], func=AF.Square, accum_out=ss[:]),
                  reads=[xt], writes=[junk, ss])
            kb.op('act', lambda e: e.activation(out=rs[:], in_=ss[:], func=AF.Sqrt, scale=1.0 / D, bias=1e-6),
                  reads=[ss], writes=[rs])
            kb.op('dve', lambda e: e.reciprocal(out=rs[:], in_=rs[:]), reads=[rs], writes=[rs])
            kb.op('dve', lambda e: e.tensor_scalar(out=xn[:], in0=xt[:], scalar1=rs[:, 0:1], scalar2=None, op0=ALU.mult),
                  reads=[xt, rs], writes=[xn])
            for half in range(2):
                pt = ptr[half]
                for j in range(4):
                    kc = half * 4 + j
                    kb.op('pe', lambda e: e.transpose(out=pt[:, j * 128:(j + 1) * 128], in_=xn[:, kc * 128:(kc + 1) * 128],
                                                      identity=ident[:]), reads=[xn, ident], writes=[pt])
                eng = 'act' if half == 0 else 'dve'
                dst = hT[:, half * 4:(half + 1) * 4, i * 128:(i + 1) * 128]
                src = pt[:].rearrange("p (a b) -> p a b", a=4)
                if eng == 'act':
                    kb.op('act', lambda e: e.activation(out=dst, in_=src, func=AF.Copy), reads=[pt], writes=[hT])
                else:
                    kb.op('dve', lambda e: e.tensor_copy(out=dst, in_=src), reads=[pt], writes=[hT])

        chunks = []
        for (nm, c0, wd) in SEGS:
            o = 0
            while o < wd:
                cw = min(512, wd - o)
                chunks.append((nm, c0 + o, cw, o))
                o += cw
        wf = [kb.sb([128, 8, 512], F32, f'wf{i}') for i in range(2)]
        wb = [kb.sb([128, 8, 512], BF16, f'wb{i}') for i in range(2)]
        stg = [kb.sb([128, 512], F32, f'stg{i}') for i in range(4)]
        stb = [kb.sb([128, 512], BF16, f'stb{i}') for i in range(2)]
        rt = [kb.sb([128, 64], F32, f'rt{i}') for i in range(6)]
        nev = 0
        for ci, (nm, c0, cw, soff) in enumerate(chunks):
            wfc, wbc = wf[ci % 2], wb[ci % 2]
            kb.dma(wfc[:, :, 0:cw], w[:, c0:c0 + cw].rearrange("(kc k) n -> k kc n", k=128), wfc, True)
            for kc in range(8):
                kb.op('pool', lambda e: e.tensor_scalar(out=wbc[:, kc, 0:cw], in0=wfc[:, kc, 0:cw], scalar1=g_sb[:, kc:kc + 1],
                                                        scalar2=None, op0=ALU.mult), reads=[wfc, g_sb], writes=[wbc])
            for i in range(nt):
                pm = pmm[nev % 4]
                for kc in range(8):
                    kb.op('pe', lambda e: e.matmul(pm[:, 0:cw], lhsT=hT[:, kc, i * 128:(i + 1) * 128], rhs=wbc[:, kc, 0:cw],
                                                   start=(kc == 0), stop=(kc == 7)), reads=[hT, wbc], writes=[pm])
                sg = stg[nev % 4]
                if nev % 2 == 0:
                    kb.op('act', lambda e: e.activation(out=sg[:, 0:cw], in_=pm[:, 0:cw], func=AF.Copy), reads=[pm], writes=[sg])
                else:
                    kb.op('dve', lambda e: e.tensor_copy(out=sg[:, 0:cw], in_=pm[:, 0:cw]), reads=[pm], writes=[sg])
                nev += 1
                if with_rope and nm in ('sq', 'sk', 'iq', 'ikw'):
                    if nm in ('sq', 'sk'):
                        nh, hd, hf, cs, sn = 4, 128, 16, cos_sa, sin_sa
                    elif nm == 'iq':
                        nh, hd, hf, cs, sn = 8, 64, 8, cos_ix, sin_ix
                    else:
                        nh, hd, hf, cs, sn = 1, 64, 8, cos_ix, sin_ix
                    v = sg[:, 0:nh * hd].rearrange("p (h d) -> p h d", h=nh)
                    x1, x2 = v[:, :, 0:hf], v[:, :, hf:2 * hf]
                    cb = cs[:, i, :].unsqueeze(1).to_broadcast([128, nh, hf])
                    sb_ = sn[:, i, :].unsqueeze(1).to_broadcast([128, nh, hf])
                    tt = [rt[k][:, 0:nh * hf].rearrange("p (h d) -> p h d", h=nh) for k in range(6)]
                    for k, (a, b) in enumerate([(x1, cb), (x2, sb_), (x2, cb), (x1, sb_)]):
                        kb.op('pool', lambda e: e.tensor_tensor(out=tt[k], in0=a, in1=b, op=ALU.mult),
                              reads=[sg, cs, sn], writes=[rt[k]])
                    kb.op('pool', lambda e: e.tensor_tensor(out=x1, in0=tt[0], in1=tt[1], op=ALU.subtract),
                          reads=[rt[0], rt[1]], writes=[sg])
                    kb.op('pool', lambda e: e.tensor_tensor(out=x2, in0=tt[2], in1=tt[3], op=ALU.add),
                          reads=[rt[2], rt[3]], writes=[sg])
                kb.dma(P[i * 128:(i + 1) * 128, c0:c0 + cw], sg[:, 0:cw], sg, False)
                if nm in ('sq', 'sk', 'sv'):
                    bo = {'sq': 0, 'sk': 1024, 'sv': 2048}[nm] + soff
                    sbt = stb[i % 2]
                    kb.op('pool', lambda e: e.tensor_copy(out=sbt[:, 0:cw], in_=sg[:, 0:cw]), reads=[sg], writes=[sbt])
                    kb.dma(Pb[i * 128:(i + 1) * 128, bo:bo + cw], sbt[:, 0:cw], sbt, False)
        kb.finish(stg + stb)
    return nc


def host_consts():
    inv_sa = (500000.0 ** (-(np.arange(16, dtype=np.float32) * 2.0 / 32))).astype(np.float32)
    inv_ix = (500000.0 ** (-(np.arange(8, dtype=np.float32) * 2.0 / 16))).astype(np.float32)
    return np.tile(inv_sa[None], (128, 1)), np.tile(inv_ix[None], (128, 1))


S_LEN = 16384
NBLK = S_LEN // 128
NEG = -30000.0


def build_B(NH=2, nblk=NBLK, NB=4, stage=9, sub=9):
    SL = nblk * 128
    nc = bass.Bass("TRN2", target_bir_lowering=False)
    xp = nc.dram_tensor("xp", [NH, SL + 3, 384], F32, kind="ExternalInput").ap()
    zin = nc.dram_tensor("z", [NH, SL, 128], F32, kind="ExternalInput").ap()
    bain = nc.dram_tensor("ba", [NH, 128, 2, nblk], F32, kind="ExternalInput").ap()
    wcin = nc.dram_tensor("wc", [NH, 128, 4, 384], F32, kind="ExternalInput").ap()
    hpin = nc.dram_tensor("hp", [NH, 128, 2], F32, kind="ExternalInput").ap()
    dnin = nc.dram_tensor("dnw", [128, 128], F32, kind="ExternalInput").ap()
    oout = nc.dram_tensor("o", [NH, SL, 128], F32, kind="ExternalOutput").ap()
    with ExitStack() as st:
        kb = KB(nc, st)
        ident, ones = make_ident(kb)
        def tri_mask(name, allowed_fill, other_fill, kind):
            m = kb.sb([128, 128], F32, name)
            kb.op('pool', lambda e: e.memset(m[:], allowed_fill), writes=[m])
            if kind == 'upper_incl':
                kb.op('pool', lambda e: e.affine_select(out=m[:], in_=m[:], pattern=[[1, 128]], compare_op=ALU.is_ge,
                                                        fill=other_fill, base=0, channel_multiplier=-1), reads=[m], writes=[m])
                kb.op('pool', lambda e: e.memset(m[0:64, 64:128], other_fill), writes=[m])
            else:
                kb.op('pool', lambda e: e.affine_select(out=m[:], in_=m[:], pattern=[[-1, 128]], compare_op=ALU.is_ge,
                                                        fill=other_fill, base=-1, channel_multiplier=1), reads=[m], writes=[m])
                kb.op('pool', lambda e: e.memset(m[64:128, 0:64], other_fill), writes=[m])
            return m
        U2 = tri_mask('U2', 1.0, 0.0, 'upper_incl')
        NEGUI = tri_mask('NEGUI', 0.0, NEG, 'upper_incl')
        NEGL = tri_mask('NEGL', 0.0, NEG, 'lower_strict')
        dnw = kb.sb([128, 128], F32, 'dnw_sb')
        kb.dma(dnw[:], dnin[:, :], dnw, True)

        banks = [kb.ps([128, 512], F32, f'bank{i}') for i in range(8)]
        pT = banks[0]
        pk = kb.view(banks[1], 0, 384, 'pk')
        ppow = [kb.view(banks[2], 0, 256, 'ppow0'), kb.view(banks[3], 0, 256, 'ppow1')]
        pR = [kb.view(banks[4], 0, 128, 'pR0'), kb.view(banks[5], 0, 128, 'pR1')]
        pwa = kb.view(banks[4], 128, 256, 'pwa')
        po2 = [kb.view(banks[4], 256, 384, 'po0'), kb.view(banks[4], 384, 512, 'po1')]
        pg = kb.view(banks[5], 128, 256, 'pg')
        pau = kb.view(banks[5], 256, 384, 'pau')
        pss = kb.view(banks[6], 0, 128, 'pss')
        pa = kb.view(banks[6], 128, 256, 'pa')
        pbig = banks[7]
        puw = kb.view(banks[7], 0, 256, 'puw')

        def alloc(n, shape, name):
            return [kb.sb(shape, F32, f'{name}{i}') for i in range(n)]

        for hh in range(NH):
            ba = kb.sb([128, 2, nblk], F32, f'ba{hh}')
            kb.dma(ba[:], bain[hh], ba, True)
            hp = kb.sb([128, 2], F32, f'hp{hh}')
            kb.dma(hp[:], hpin[hh], hp, True)
            wc = kb.sb([128, 4, 384], F32, f'wc{hh}')
            kb.dma(wc[:], wcin[hh], wc, True)
            beta = kb.sb([128, nblk], F32, f'beta{hh}')
            kb.op('act', lambda e: e.activation(out=beta[:], in_=ba[:, 0, :], func=AF.Sigmoid), reads=[ba], writes=[beta])
            nega = kb.sb([128, 1], F32, f'nega{hh}')
            kb.op('act', lambda e: e.activation(out=nega[:], in_=hp[:, 0:1], func=AF.Exp), reads=[hp], writes=[nega])
            kb.op('dve', lambda e: e.tensor_scalar(out=nega[:], in0=nega[:], scalar1=-1.0, scalar2=None, op0=ALU.mult),
                  reads=[nega], writes=[nega])
            g = kb.sb([128, nblk], F32, f'g{hh}')
            kb.op('act', lambda e: e.activation(out=g[:], in_=ba[:, 1, :], func=AF.Exp, bias=hp[:, 1:2]), reads=[ba, hp], writes=[g])
            kb.op('act', lambda e: e.activation(out=g[:], in_=g[:], func=AF.Ln, bias=1.0), reads=[g], writes=[g])
            kb.op('dve', lambda e: e.tensor_scalar(out=g[:], in0=g[:], scalar1=nega[:, 0:1], scalar2=None, op0=ALU.mult),
                  reads=[g, nega], writes=[g])
            kb.op('pe', lambda e: e.matmul(pbig[:, 0:nblk], lhsT=U2[:], rhs=g[:], start=True, stop=True), reads=[U2, g], writes=[pbig])
            gc = kb.sb([128, nblk], F32, f'gc{hh}')
            kb.op('dve', lambda e: e.tensor_copy(out=gc[:], in_=pbig[:, 0:nblk]), reads=[pbig], writes=[gc])
            g2 = kb.sb([128, 2, nblk], F32, f'g2{hh}')
            kb.op('pool', lambda e: e.memset(g2[:], 0.0), writes=[g2])
            kb.op('pool', lambda e: e.tensor_copy(out=g2[0:64, 0, :], in_=g[0:64, :]), reads=[g], writes=[g2])
            kb.op('pool', lambda e: e.tensor_copy(out=g2[64:128, 1, :], in_=g[64:128, :]), reads=[g], writes=[g2])
            kb.op('pe', lambda e: e.matmul(pbig[:, 0:2 * nblk], lhsT=ones[:], rhs=g2[:].rearrange("p a b -> p (a b)"),
                                           start=True, stop=True), reads=[ones, g2], writes=[pbig])
            glB = kb.sb([128, 2, nblk], F32, f'glB{hh}')
            kb.op('dve', lambda e: e.tensor_copy(out=glB[:].rearrange("p a b -> p (a b)"), in_=pbig[:, 0:2 * nblk]),
                  reads=[pbig], writes=[glB])
            cd = kb.sb([128, 2, nblk], F32, f'cd{hh}')
            kb.op('act', lambda e: e.activation(out=cd[:], in_=glB[:], func=AF.Exp), reads=[glB], writes=[cd])
            ekd = kb.sb([128, nblk], F32, f'ekd{hh}')
            kb.op('dve', lambda e: e.tensor_tensor(out=ekd[0:64, :], in0=glB[0:64, 0, :], in1=gc[0:64, :], op=ALU.subtract),
                  reads=[glB, gc], writes=[ekd])
            kb.op('dve', lambda e: e.tensor_tensor(out=ekd[64:128, :], in0=glB[64:128, 1, :], in1=gc[64:128, :], op=ALU.subtract),
                  reads=[glB, gc], writes=[ekd])
            kb.op('act', lambda e: e.activation(out=ekd[:], in_=ekd[:], func=AF.Exp), reads=[ekd], writes=[ekd])
            eg = kb.sb([128, nblk], F32, f'eg{hh}')
            kb.op('act', lambda e: e.activation(out=eg[:], in_=gc[:], func=AF.Exp), reads=[gc], writes=[eg])
            beg = kb.sb([128, nblk], F32, f'beg{hh}')
            kb.op('dve', lambda e: e.tensor_tensor(out=beg[:], in0=beta[:], in1=eg[:], op=ALU.mult), reads=[beta, eg], writes=[beg])

            S = alloc(2, [128, 128], f'S{hh}_')
            kb.op('pool', lambda e: e.memset(S[0][:], 0.0), writes=[S[0]])
            scur = 0

            if hh == 0:
                X = alloc(4, [128, NB, 384], 'X')
                y = kb.sb([128, NB, 384], F32, 'y')
                tmp = kb.sb([128, NB, 384], F32, 'tmp')
                sq = kb.sb([128, NB, 2, 128], F32, 'sq')
                ssn = kb.sb([128, NB, 2], F32, 'ssn')
                zt = kb.sb([128, NB, 128], F32, 'zt')
                zw = kb.sb([128, NB, 128], F32, 'zw')
                kbt = kb.sb([128, NB, 128], F32, 'kbt')
                rw = kb.sb([128, NB, 256], F32, 'rw')
                kdt = kb.sb([128, NB, 128], F32, 'kdt')
                kdm = kb.sb([128, NB, 2, 128], F32, 'kdm')
                kb.op('pool', lambda e: e.memset(kdm[:], 0.0), writes=[kdm])
                qdt = kb.sb([128, NB, 128], F32, 'qdt')
                TT = alloc(NB, [128, 4, 128], 'TT')
                dg = alloc(NB, [128, 128], 'dg')
                aL = alloc(NB, [128, 128], 'aL')
                aU = alloc(NB, [128, 128], 'aU')
                DU = alloc(NB, [128, 128], 'DU')
                Mm = alloc(NB, [128, 128], 'Mm')
                Nm = alloc(NB, [128, 128], 'Nm')
                ATm = alloc(NB, [128, 128], 'ATm')
                Rm = [alloc(NB, [128, 128], f'R{k}_') for k in range(2)]
                PW = [alloc(NB, [128, 256], f'PW{k}_') for k in range(2)]
                uw = alloc(NB, [128, 256], 'uw')
                QpT = alloc(NB, [128, 128], 'QpT')
                au = alloc(NB, [128, 128], 'au')
                AcT = alloc(2 * NB, [128, 128], 'AcT')
                osb = alloc(NB, [128, 128], 'osb')
                junk = kb.sb([128, 128], F32, 'junk')
                oss = alloc(NB, [128, 1], 'oss')
                ofin = alloc(NB, [128, 128], 'ofin')

            for g0 in range(0, nblk if stage >= 2 else 0, NB):
                for j in range(4):
                    src = xp[hh, j + g0 * 128: j + (g0 + NB) * 128, :].rearrange("(b p) c -> p b c", p=128)
                    kb.dma(X[j][:], src, X[j], True)
                kb.dma(zt[:], zin[hh, g0 * 128:(g0 + NB) * 128, :].rearrange("(b p) c -> p b c", p=128), zt, True)
                def wb(j):
                    return wc[:, j, :].unsqueeze(1).to_broadcast([128, NB, 384])
                kb.op('dve', lambda e: e.tensor_tensor(out=y[:], in0=X[0][:], in1=wb(0), op=ALU.mult), reads=[X[0], wc], writes=[y])
                for j in range(1, 4):
                    kb.op('pool', lambda e: e.tensor_tensor(out=tmp[:], in0=X[j][:], in1=wb(j), op=ALU.mult), reads=[X[j], wc], writes=[tmp])
                    kb.op('dve', lambda e: e.tensor_tensor(out=y[:], in0=y[:], in1=tmp[:], op=ALU.add), reads=[y, tmp], writes=[y])
                kb.op('act', lambda e: e.activation(out=y[:], in_=y[:], func=AF.Silu), reads=[y], writes=[y])
                yv = y[:].rearrange("p b (t d) -> p b t d", t=3)
                kb.op('pool', lambda e: e.tensor_tensor(out=sq[:], in0=yv[:, :, 0:2, :], in1=yv[:, :, 0:2, :], op=ALU.mult), reads=[y], writes=[sq])
                kb.op('dve', lambda e: e.tensor_reduce(out=ssn[:], in_=sq[:], axis=AX.X, op=ALU.add), reads=[sq], writes=[ssn])
                kb.op('act', lambda e: e.activation(out=ssn[:], in_=ssn[:], func=AF.Sqrt, bias=1e-6), reads=[ssn], writes=[ssn])
                kb.op('dve', lambda e: e.reciprocal(out=ssn[:], in_=ssn[:]), reads=[ssn], writes=[ssn])
                kb.op('dve', lambda e: e.tensor_scalar(out=ssn[:, :, 0:1], in0=ssn[:, :, 0:1], scalar1=128.0 ** -0.5, scalar2=None, op0=ALU.mult),
                      reads=[ssn], writes=[ssn])
                kb.op('dve', lambda e: e.tensor_tensor(out=yv[:, :, 0:2, :], in0=yv[:, :, 0:2, :],
                                                       in1=ssn[:].unsqueeze(3).to_broadcast([128, NB, 2, 128]), op=ALU.mult),
                      reads=[y, ssn], writes=[y])
                qn, kn, vv = yv[:, :, 0, :], yv[:, :, 1, :], yv[:, :, 2, :]
                def bc(tab):
                    return tab[:, g0:g0 + NB].unsqueeze(2).to_broadcast([128, NB, 128])
                kb.op('pool', lambda e: e.tensor_tensor(out=kbt[:], in0=kn, in1=bc(beta), op=ALU.mult), reads=[y, beta], writes=[kbt])
                kb.op('dve', lambda e: e.tensor_tensor(out=rw[:, :, 0:128], in0=vv, in1=bc(beta), op=ALU.mult), reads=[y, beta], writes=[rw])
                kb.op('pool', lambda e: e.tensor_tensor(out=rw[:, :, 128:256], in0=kn, in1=bc(beg), op=ALU.mult), reads=[y, beg], writes=[rw])
                kb.op('dve', lambda e: e.tensor_tensor(out=kdt[:], in0=kn, in1=bc(ekd), op=ALU.mult), reads=[y, ekd], writes=[kdt])
                kb.op('pool', lambda e: e.tensor_tensor(out=qdt[:], in0=qn, in1=bc(eg), op=ALU.mult), reads=[y, eg], writes=[qdt])
                kb.op('pool', lambda e: e.tensor_copy(out=kdm[0:64, :, 0, :], in_=kdt[0:64, :, :]), reads=[kdt], writes=[kdm])
                kb.op('pool', lambda e: e.tensor_copy(out=kdm[64:128, :, 1, :], in_=kdt[64:128, :, :]), reads=[kdt], writes=[kdm])
                kb.op('act', lambda e: e.activation(out=zw[:], in_=zt[:], func=AF.Silu), reads=[zt], writes=[zw])
                kb.op('pool', lambda e: e.tensor_tensor(out=zw[:], in0=zw[:], in1=dnw[:].unsqueeze(1).to_broadcast([128, NB, 128]), op=ALU.mult),
                      reads=[zw, dnw], writes=[zw])
                for b in range(NB if stage >= 3 else 0):
                    blk = g0 + b
                    for j, (srcT, sap) in enumerate([(y, kn[:, b, :]), (kbt, kbt[:, b, :]), (y, qn[:, b, :]), (qdt, qdt[:, b, :])]):
                        kb.op('pe', lambda e: e.transpose(out=pT[:, j * 128:(j + 1) * 128], in_=sap, identity=ident[:]),
                              reads=[srcT, ident], writes=[pT])
                    kb.op('act', lambda e: e.activation(out=TT[b][:].rearrange("p a b -> p (a b)"), in_=pT[:], func=AF.Copy),
                          reads=[pT], writes=[TT[b]])
                    kT, kbT, qT, qdT = (TT[b][:, j, :] for j in range(4))
                    if sub < 2:
                        continue
                    kb.op('pool', lambda e: e.tensor_scalar(out=dg[b][:], in0=ident[:], scalar1=gc[:, blk:blk + 1], scalar2=None, op0=ALU.mult),
                          reads=[ident, gc], writes=[dg[b]])
                    kb.op('pe', lambda e: e.matmul(pg[:], lhsT=ones[:], rhs=dg[b][:], start=True, stop=True), reads=[ones, dg[b]], writes=[pg])
                    kb.op('dve', lambda e: e.tensor_scalar(out=aU[b][:], in0=pg[:], scalar1=gc[:, blk:blk + 1], scalar2=None, op0=ALU.subtract),
                          reads=[pg, gc], writes=[aU[b]])
                    kb.op('pool', lambda e: e.tensor_scalar(out=aL[b][:], in0=aU[b][:], scalar1=-1.0, scalar2=None, op0=ALU.mult),
                          reads=[aU[b]], writes=[aL[b]])
                    kb.op('pool', lambda e: e.tensor_tensor(out=aL[b][:], in0=aL[b][:], in1=NEGL[:], op=ALU.add), reads=[aL[b], NEGL], writes=[aL[b]])
                    kb.op('pool', lambda e: e.tensor_tensor(out=aU[b][:], in0=aU[b][:], in1=NEGUI[:], op=ALU.add), reads=[aU[b], NEGUI], writes=[aU[b]])
                    kb.op('act', lambda e: e.activation(out=aL[b][:], in_=aL[b][:], func=AF.Exp), reads=[aL[b]], writes=[aL[b]])
                    kb.op('act', lambda e: e.activation(out=aU[b][:], in_=aU[b][:], func=AF.Exp), reads=[aU[b]], writes=[aU[b]])
                    kb.op('pool', lambda e: e.tensor_tensor(out=DU[b][:], in0=aU[b][:], in1=ident[:], op=ALU.subtract), reads=[aU[b], ident], writes=[DU[b]])
                    if sub < 3:
                        continue
                    kb.op('pe', lambda e: e.matmul(pk[:, 0:128], lhsT=kbT, rhs=kT, start=True, stop=True), reads=[TT[b]], writes=[pk])
                    kb.op('pe', lambda e: e.matmul(pk[:, 128:256], lhsT=kT, rhs=kbT, start=True, stop=True), reads=[TT[b]], writes=[pk])
                    kb.op('pe', lambda e: e.matmul(pk[:, 256:384], lhsT=kT, rhs=qT, start=True, stop=True), reads=[TT[b]], writes=[pk])
                    if sub < 4:
                        continue
                    kb.op('dve', lambda e: e.tensor_tensor(out=Mm[b][:], in0=pk[:, 0:128], in1=aL[b][:], op=ALU.mult), reads=[pk, aL[b]], writes=[Mm[b]])
                    kb.op('dve', lambda e: e.tensor_tensor(out=Nm[b][:], in0=pk[:, 128:256], in1=DU[b][:], op=ALU.mult), reads=[pk, DU[b]], writes=[Nm[b]])
                    kb.op('dve', lambda e: e.tensor_tensor(out=ATm[b][:], in0=pk[:, 256:384], in1=aU[b][:], op=ALU.mult), reads=[pk, aU[b]], writes=[ATm[b]])
                    kb.op('pool', lambda e: e.tensor_tensor(out=Rm[0][b][:], in0=ident[:], in1=Nm[b][:], op=ALU.subtract), reads=[ident, Nm[b]], writes=[Rm[0][b]])
                curM = [Mm[b][:] for b in range(NB)]
                curN = [Nm[b][:] for b in range(NB)]
                curMt = [Mm[b] for b in range(NB)]
                curNt = [Nm[b] for b in range(NB)]
                rcur = 0
                for lvl in range(5 if stage >= 4 else 0):
                    last = (lvl == 4)
                    pw = PW[lvl % 2]
                    for b in range(NB):
                        pp = ppow[b % 2]
                        kb.op('pe', lambda e: e.matmul(pp[:, 0:128], lhsT=curN[b], rhs=curM[b], start=True, stop=True),
                              reads=[curMt[b], curNt[b]], writes=[pp])
                        if not last:
                            kb.op('pe', lambda e: e.matmul(pp[:, 128:256], lhsT=curM[b], rhs=curN[b], start=True, stop=True),
                                  reads=[curMt[b], curNt[b]], writes=[pp])
                        wd = 128 if last else 256
                        if b % 2 == 0:
                            kb.op('act', lambda e: e.activation(out=pw[b][:, 0:wd], in_=pp[:, 0:wd], func=AF.Copy), reads=[pp], writes=[pw[b]])
                        else:
                            kb.op('dve', lambda e: e.tensor_copy(out=pw[b][:, 0:wd], in_=pp[:, 0:wd]), reads=[pp], writes=[pw[b]])
                    for b in range(NB):
                        pr = pR[b % 2]
                        kb.op('pe', lambda e: e.matmul(pr[:], lhsT=pw[b][:, 0:128], rhs=Rm[rcur][b][:], start=True, stop=True),
                              reads=[pw[b], Rm[rcur][b]], writes=[pr])
                        kb.op('dve', lambda e: e.tensor_tensor(out=Rm[1 - rcur][b][:], in0=pr[:], in1=Rm[rcur][b][:], op=ALU.add),
                              reads=[pr, Rm[rcur][b]], writes=[Rm[1 - rcur][b]])
                    rcur = 1 - rcur
                    curM = [pw[b][:, 0:128] for b in range(NB)]
                    curN = [pw[b][:, 128:256] for b in range(NB)]
                    curMt = [pw[b] for b in range(NB)]
                    curNt = [pw[b] for b in range(NB)]
                for b in range(NB if stage >= 5 else 0):
                    blk = g0 + b
                    R5 = Rm[rcur][b]
                    kb.op('pe', lambda e: e.matmul(puw[:], lhsT=R5[:], rhs=rw[:, b, :], start=True, stop=True), reads=[R5, rw], writes=[puw])
                    kb.op('act', lambda e: e.activation(out=uw[b][:], in_=puw[:], func=AF.Copy), reads=[puw], writes=[uw[b]])
                    u_, w_ = uw[b][:, 0:128], uw[b][:, 128:256]
                    kb.op('pe', lambda e: e.matmul(pwa[:], lhsT=w_, rhs=ATm[b][:], start=True, stop=True), reads=[uw[b], ATm[b]], writes=[pwa])
                    kb.op('dve', lambda e: e.tensor_tensor(out=QpT[b][:], in0=TT[b][:, 3, :], in1=pwa[:], op=ALU.subtract),
                          reads=[TT[b], pwa], writes=[QpT[b]])
                    kb.op('pe', lambda e: e.matmul(pau[:], lhsT=ATm[b][:], rhs=u_, start=True, stop=True), reads=[ATm[b], uw[b]], writes=[pau])
                    kb.op('act', lambda e: e.activation(out=au[b][:], in_=pau[:], func=AF.Copy), reads=[pau], writes=[au[b]])
                    for c in range(2):
                        r0, r1 = c * 64, (c + 1) * 64
                        kb.op('pe', lambda e: e.matmul(pa[:], lhsT=uw[b][:, 128:256], rhs=kdm[:, b, c, :], start=True, stop=True),
                              reads=[uw[b], kdm], writes=[pa])
                        kb.op('dve', lambda e: e.scalar_tensor_tensor(out=AcT[2 * b + c][:], in0=ident[:], scalar=cd[:, c, blk:blk + 1], in1=pa[:],
                                                                      op0=ALU.mult, op1=ALU.subtract),
                              reads=[ident, cd, pa], writes=[AcT[2 * b + c]])
                for b in range(NB if stage >= 6 else 0):
                    for c in range(2):
                        r0, r1 = c * 64, (c + 1) * 64
                        Sc, Sn = S[scur], S[1 - scur]
                        kb.op('pe', lambda e: e.matmul(po2[c][:], lhsT=QpT[b][:], rhs=Sc[:], start=True, stop=True),
                              reads=[QpT[b], Sc], writes=[po2[c]])
                        kb.op('pe', lambda e: e.matmul(pss[:], lhsT=AcT[2 * b + c][:], rhs=Sc[:], start=True, stop=False),
                              reads=[AcT[2 * b + c], Sc], writes=[pss])
                        kb.op('pe', lambda e: e.matmul(pss[:], lhsT=kdm[:, b, c, :], rhs=uw[b][:, 0:128], start=False, stop=True),
                              reads=[kdm, uw[b]], writes=[pss])
                        kb.op('act', lambda e: e.activation(out=Sn[:], in_=pss[:], func=AF.Copy), reads=[pss], writes=[Sn])
                        scur = 1 - scur
                    kb.op('dve', lambda e: e.tensor_tensor(out=osb[b][0:64, :], in0=po2[0][0:64, :], in1=au[b][0:64, :], op=ALU.add), reads=[po2[0], au[b]], writes=[osb[b]])
                    kb.op('dve', lambda e: e.tensor_tensor(out=osb[b][64:128, :], in0=po2[1][64:128, :], in1=au[b][64:128, :], op=ALU.add), reads=[po2[1], au[b]], writes=[osb[b]])
                    kb.op('act', lambda e: e.activation(out=junk[:], in_=osb[b][:], func=AF.Square, accum_out=oss[b][:]),
                          reads=[osb[b]], writes=[junk, oss[b]])
                    kb.op('act', lambda e: e.activation(out=oss[b][:], in_=oss[b][:], func=AF.Sqrt, scale=1.0 / 128, bias=1e-6),
                          reads=[oss[b]], writes=[oss[b]])
                    kb.op('dve', lambda e: e.reciprocal(out=oss[b][:], in_=oss[b][:]), reads=[oss[b]], writes=[oss[b]])
                    kb.op('dve', lambda e: e.scalar_tensor_tensor(out=ofin[b][:], in0=osb[b][:], scalar=oss[b][:, 0:1], in1=zw[:, b, :],
                                                                  op0=ALU.mult, op1=ALU.mult), reads=[osb[b], oss[b], zw], writes=[ofin[b]])
                    blk = g0 + b
                    kb.dma(oout[hh, blk * 128:(blk + 1) * 128, :], ofin[b][:], ofin[b], False)
        kb.finish(ofin)
    return nc


NEGB = -30000.0
NIT = 20
SEQ = 16384


def build_C(NQB=32):
    NQ = NQB * 128
    nc = bass.Bass("TRN2", target_bir_lowering=False)
    iqT = nc.dram_tensor("iqT", [64, 8, NQ], F32, kind="ExternalInput").ap()
    iwin = nc.dram_tensor("iw", [128, NQB, 8], F32, kind="ExternalInput").ap()
    ikT = nc.dram_tensor("ikT", [64, SEQ], F32, kind="ExternalInput").ap()
    qTin = nc.dram_tensor("qT", [128, 8, NQ], BF16, kind="ExternalInput").ap()
    kTin = nc.dram_tensor("kT", [8, 128, SEQ], BF16, kind="ExternalInput").ap()
    vin = nc.dram_tensor("v", [8, 128, SEQ // 128, 128], BF16, kind="ExternalInput").ap()
    admin = nc.dram_tensor("admis", [128, 512], F32, kind="ExternalInput").ap()
    oout = nc.dram_tensor("o", [NQ, 1024], F32, kind="ExternalOutput").ap()
    with ExitStack() as st:
        kb = KB(nc, st)
        identf, _ = make_ident(kb)
        negI = kb.sb([128, 128], BF16, 'negI')
        kb.op('dve', lambda e: e.tensor_scalar(out=negI[:], in0=identf[:], scalar1=NEGB, scalar2=None, op0=ALU.mult),
              reads=[identf], writes=[negI])
        admis = kb.sb([128, 512], F32, 'admis_sb')
        kb.dma(admis[:], admin[:, :], admis, True)
        pow2 = kb.sb([128, NIT], F32, 'pow2')
        for k in range(NIT):
            kb.op('pool', lambda e: e.memset(pow2[:, k:k + 1], 2.0 ** -(k + 1)), writes=[pow2])
        iw = kb.sb([128, NQB, 8], F32, 'iw_sb')
        kb.dma(iw[:], iwin[:, :, :], iw, True)
        kb.op('dve', lambda e: e.tensor_scalar(out=iw[:], in0=iw[:], scalar1=(8 ** -0.5) * (64 ** -0.5), scalar2=None, op0=ALU.mult),
              reads=[iw], writes=[iw])
        score = kb.sb([128, SEQ], F32, 'score')
        mask = kb.sb([128, SEQ], BF16, 'mask')
        ikc = [kb.sb([64, 2048], F32, f'ikc{i}') for i in range(2)]
        kTc = [kb.sb([128, 2048], BF16, f'kTc{i}') for i in range(2)]
        Vc = [kb.sb([128, 16, 129], BF16, f'Vc{i}') for i in range(2)]
        for i in range(2):
            kb.op('pool', lambda e: e.memset(Vc[i][:], 1.0), writes=[Vc[i]])
        iq = kb.sb([64, 8, 128], F32, 'iq_sb')
        qT = kb.sb([128, 8, 128], BF16, 'qT_sb')
        rel = [kb.sb([128, 512], F32, f'rel{i}') for i in range(3)]
        PT = [kb.sb([128, 4, 128], BF16, f'PT{i}') for i in range(2)]
        obuf = [kb.sb([128, 8, 128], F32, f'obuf{i}') for i in range(2)]
        sm = {n: kb.sb([128, 1], F32, 'sm_' + n) for n in ('A', 'lo', 'rng', 'mid', 'cnt', 't', 'rec')}
        steps = kb.sb([128, NIT], F32, 'steps')
        pidx = [kb.ps([128, 512], F32, f'pidx{i}') for i in range(4)]
        pqk = [kb.ps([128, 512], F32, f'pqk{i}') for i in range(2)]
        pacc = kb.ps([128, 512], F32, 'pacc')
        maskd = [mask, kb.sb([128, SEQ], BF16, 'mask1')]
        oraw = [kb.sb([128, 8, 129], F32, f'oraw{i}') for i in range(2)]
        rec8 = kb.sb([128, 8], F32, 'rec8')
        pacc2 = [pacc, kb.ps([128, 512], F32, 'pacc1')]
        ctr = {'idx': 0, 'rel': 0, 'qk': 0, 'kv': 0, 'ik': 0, 'acc': 0}

        def emit_idx(k):
            L = 512 * (k + 1)
            kb.dma(iq[:], iqT[:, :, k * 128:(k + 1) * 128], iq, True)
            for c0 in range(0, L, 2048):
                cw = min(2048, L - c0)
                ik = ikc[ctr['ik'] % 2]
                ctr['ik'] += 1
                kb.dma(ik[:, 0:cw], ikT[:, c0:c0 + cw], ik, True)
                for s0 in range(0, cw, 512):
                    for h in range(8):
                        pi = pidx[ctr['idx'] % 4]
                        ctr['idx'] += 1
                        kb.op('pe', lambda e: e.matmul(pi[:], lhsT=iq[:, h, :], rhs=ik[:, s0:s0 + 512], start=True, stop=True),
                              reads=[iq, ik], writes=[pi])
                        r = rel[ctr['rel'] % 3]
                        ctr['rel'] += 1
                        kb.op('act', lambda e: e.activation(out=r[:], in_=pi[:], func=AF.Relu), reads=[pi], writes=[r])
                        dst = score[:, c0 + s0:c0 + s0 + 512]
                        if h == 0:
                            kb.op('dve', lambda e: e.tensor_scalar(out=dst, in0=r[:], scalar1=iw[:, k, 0:1], scalar2=None, op0=ALU.mult),
                                  reads=[r, iw], writes=[score])
                        else:
                            kb.op('dve', lambda e: e.scalar_tensor_tensor(out=dst, in0=r[:], scalar=iw[:, k, h:h + 1], in1=dst,
                                                                          op0=ALU.mult, op1=ALU.add), reads=[r, iw, score], writes=[score])

        def emit_bis(k):
            L = 512 * (k + 1)
            mk = maskd[k % 2]
            A, lo, rng, mid, cnt, tt, rec = (sm[n] for n in ('A', 'lo', 'rng', 'mid', 'cnt', 't', 'rec'))
            kb.op('dve', lambda e: e.tensor_reduce(out=A[:], in_=score[:, 0:L], axis=AX.X, op=ALU.max, apply_absolute_value=True),
                  reads=[score], writes=[A])
            kb.op('dve', lambda e: e.tensor_scalar(out=lo[:], in0=A[:], scalar1=-1.0, scalar2=-1.0, op0=ALU.mult, op1=ALU.add),
                  reads=[A], writes=[lo])
            kb.op('dve', lambda e: e.tensor_scalar(out=rng[:], in0=A[:], scalar1=2.0, scalar2=2.0, op0=ALU.mult, op1=ALU.add),
                  reads=[A], writes=[rng])
            kb.op('dve', lambda e: e.tensor_scalar(out=steps[:], in0=pow2[:], scalar1=rng[:, 0:1], scalar2=None, op0=ALU.mult),
                  reads=[pow2, rng], writes=[steps])
            kb.op('pool', lambda e: e.tensor_tensor(out=score[:, L - 512:L], in0=score[:, L - 512:L], in1=admis[:], op=ALU.add),
                  reads=[score, admis], writes=[score])
            for it in range(NIT):
                kb.op('dve', lambda e: e.tensor_tensor(out=mid[:], in0=lo[:], in1=steps[:, it:it + 1], op=ALU.add),
                      reads=[lo, steps], writes=[mid])
                kb.op('dve', lambda e: e.tensor_scalar(out=mk[:, 0:L], in0=score[:, 0:L], scalar1=mid[:, 0:1], scalar2=None,
                                                       op0=ALU.is_ge, op1=ALU.add, accum_out=cnt[:]),
                      reads=[score, mid], writes=[mk, cnt])
                kb.op('dve', lambda e: e.tensor_scalar(out=tt[:], in0=cnt[:], scalar1=255.5, scalar2=None, op0=ALU.is_ge),
                      reads=[cnt], writes=[tt])
                kb.op('dve', lambda e: e.tensor_tensor(out=tt[:], in0=tt[:], in1=steps[:, it:it + 1], op=ALU.mult),
                      reads=[tt, steps], writes=[tt])
                kb.op('dve', lambda e: e.tensor_tensor(out=lo[:], in0=lo[:], in1=tt[:], op=ALU.add), reads=[lo, tt], writes=[lo])
            kb.op('dve', lambda e: e.tensor_scalar(out=mk[:, 0:L], in0=score[:, 0:L], scalar1=lo[:, 0:1], scalar2=None, op0=ALU.is_lt),
                  reads=[score, lo], writes=[mk])

        def emit_attn(k):
            L = 512 * (k + 1)
            mk = maskd[k % 2]
            orw = oraw[k % 2]
            ntile = L // 128
            kb.dma(qT[:], qTin[:, :, k * 128:(k + 1) * 128], qT, True)
            for h in range(8):
                pac = pacc2[ctr['acc'] % 2]
                ctr['acc'] += 1
                for c0 in range(0, L, 2048):
                    cw = min(2048, L - c0)
                    kt, vc = kTc[ctr['kv'] % 2], Vc[ctr['kv'] % 2]
                    ctr['kv'] += 1
                    kb.dma(kt[:, 0:cw], kTin[h, :, c0:c0 + cw], kt, True)
                    kb.dma(vc[:, 0:cw // 128, 0:128], vin[h, :, c0 // 128:(c0 + cw) // 128, :], vc, True)
                    for g0 in range(0, cw, 512):
                        pq = pqk[ctr['qk'] % 2]
                        pt = PT[ctr['qk'] % 2]
                        ctr['qk'] += 1
                        for j in range(4):
                            s = c0 + g0 + j * 128
                            kb.op('pe', lambda e: e.matmul(pq[:, j * 128:(j + 1) * 128], lhsT=kt[:, g0 + j * 128:g0 + (j + 1) * 128], rhs=qT[:, h, :],
                                                           start=True, stop=False), reads=[kt, qT], writes=[pq])
                            kb.op('pe', lambda e: e.matmul(pq[:, j * 128:(j + 1) * 128], lhsT=mk[:, s:s + 128], rhs=negI[:],
                                                           start=False, stop=True), reads=[mk, negI], writes=[pq])
                        kb.op('act', lambda e: e.activation(out=pt[:].rearrange("p a b -> p (a b)"), in_=pq[:], func=AF.Exp, scale=128.0 ** -0.5),
                              reads=[pq], writes=[pt])
                        for j in range(4):
                            ti = (c0 + g0) // 128 + j
                            kb.op('pe', lambda e: e.matmul(pac[:, 0:129], lhsT=pt[:, j, :], rhs=vc[:, (g0 // 128) + j, :],
                                                           start=(ti == 0), stop=(ti == ntile - 1)), reads=[pt, vc], writes=[pac])
                kb.op('act', lambda e: e.activation(out=orw[:, h, :], in_=pac[:, 0:129], func=AF.Copy), reads=[pac], writes=[orw])

        def emit_norm(k):
            orw = oraw[k % 2]
            ob = obuf[k % 2]
            kb.op('dve', lambda e: e.reciprocal(out=rec8[:], in_=orw[:, :, 128]), reads=[orw], writes=[rec8])
            kb.op('dve', lambda e: e.tensor_tensor(out=ob[:], in0=orw[:, :, 0:128], in1=rec8[:].unsqueeze(2).to_broadcast([128, 8, 128]), op=ALU.mult),
                  reads=[orw, rec8], writes=[ob])
            kb.dma(oout[k * 128:(k + 1) * 128, :], ob[:].rearrange("p a b -> p (a b)"), ob, False)

        emit_idx(0)
        emit_bis(0)
        for k in range(NQB):
            if k + 1 < NQB:
                emit_idx(k + 1)
                emit_bis(k + 1)
            emit_attn(k)
            emit_norm(k)
        kb.finish(obuf)
    return nc


def admis_mask(j):
    m = np.zeros((128, 512), np.float32)
    o = np.arange(512)[None, :]
    lim = np.where(np.arange(128)[:, None] < 64, 128 * j + 64, 128 * j + 128)
    m[o >= lim] = NEGB
    return m


D = 1024
DFF = 2816


def build_D(NT=4096, final=False):
    nt = NT // 128
    nc = bass.Bass("TRN2", target_bir_lowering=False)
    def din(name, shape, dt=F32):
        return nc.dram_tensor(name, shape, dt, kind="ExternalInput").ap()
    x = din("x", [NT, D]); xq = din("xq", [NT, D]); gl = din("gl", [NT, 3 * D]); odn = din("odn", [NT, D]); osa = din("osa", [NT, D])
    mem = din("mem", [256, D]); gmem = din("gmem", [128, 8]); wkv = din("wkv", [D, 2 * D])
    wbr = din("wbr", [3, D, D]); wout = din("wout", [D, D]); bg = din("bg", [128, 3 * D])
    gffn = din("gffn", [128, 8]); wfi = din("wfi", [D, 2 * DFF]); wfo = din("wfo", [DFF, D]); gfin = din("gfin", [128, D])
    y = nc.dram_tensor("y", [NT, D], F32, kind="ExternalOutput").ap()
    with ExitStack() as st:
        kb = KB(nc, st)
        ident, ones = make_ident(kb)
        ones_b = kb.sb([128, 1], BF16, 'ones_b')
        kb.op('pool', lambda e: e.memset(ones_b[:], 1.0), writes=[ones_b])
        banks = [kb.ps([128, 512], F32, f'bank{i}') for i in range(8)]
        wf = [kb.sb([128, 22, 256], F32, f'wf{i}') for i in range(2)]
        wb = [kb.sb([128, 22, 256], BF16, f'wb{i}') for i in range(2)]
        cnt = {'w': 0, 'tp': 0, 'mm': 0}
        ones_g = kb.sb([128, 22], F32, 'ones_g')
        kb.op('pool', lambda e: e.memset(ones_g[:], 1.0), writes=[ones_g])

        def small_in(ap_, shape, name):
            t = kb.sb(shape, F32, name)
            kb.dma(t[:], ap_, t, True)
            return t
        gmem_sb = small_in(gmem[:, :], [128, 8], 'gmem_sb')
        gffn_sb = small_in(gffn[:, :], [128, 8], 'gffn_sb')
        bg_sb = small_in(bg[:, :], [128, 3 * D], 'bg_sb')
        gfin_sb = small_in(gfin[:, :], [128, D], 'gfin_sb') if final else None

        def transp(src, KC, dst):
            for c0 in range(0, KC, 4):
                n = min(4, KC - c0)
                pt = banks[cnt['tp'] % 2]
                cnt['tp'] += 1
                for j in range(n):
                    kb.op('pe', lambda e: e.transpose(out=pt[:, j * 128:(j + 1) * 128], in_=src[:, (c0 + j) * 128:(c0 + j + 1) * 128], identity=ident[:]),
                          reads=[src, ident], writes=[pt])
                kb.op('act', lambda e: e.activation(out=dst[:, c0:c0 + n, :], in_=pt[:, 0:n * 128].rearrange("p (a b) -> p a b", a=n), func=AF.Copy),
                      reads=[pt], writes=[dst])

        scratch = {}
        conv_tiles = []

        def prep_w(key, W, KC, N, gcol=None):
            sc_t = nc.dram_tensor("wsc_" + key, [128, KC, N], BF16, kind="Internal").ap()
            scratch[key] = sc_t
            for c0 in range(0, N, 256):
                cw = min(256, N - c0)
                i = cnt['w'] % 2
                cnt['w'] += 1
                kb.dma(wf[i][:, 0:KC, 0:cw], W[:, c0:c0 + cw].rearrange("(kc k) n -> k kc n", k=128), wf[i], True)
                for kc in range(KC):
                    sc = gcol[:, kc:kc + 1] if gcol is not None else ones_g[:, kc:kc + 1]
                    kb.op('pool', lambda e: e.tensor_scalar(out=wb[i][:, kc, 0:cw], in0=wf[i][:, kc, 0:cw], scalar1=sc, scalar2=None, op0=ALU.mult),
                          reads=[wf[i], gcol if gcol is not None else ones_g], writes=[wb[i]])
                kb.dma(sc_t[:, :, c0:c0 + cw], wb[i][:, 0:KC, 0:cw], wb[i], False)

        def lin(hT, KC, key, c0, cw):
            i = cnt['w'] % 2
            cnt['w'] += 1
            kb.dma(wb[i][:, 0:KC, 0:cw], scratch[key][:, :, c0:c0 + cw], wb[i], True)
            pm = banks[2 + cnt['mm'] % 4]
            cnt['mm'] += 1
            for kc in range(KC):
                kb.op('pe', lambda e: e.matmul(pm[:, 0:cw], lhsT=hT[:, kc, :], rhs=wb[i][:, kc, 0:cw], start=(kc == 0), stop=(kc == KC - 1)),
                      reads=[hT, wb[i]], writes=[pm])
            return pm

        def rms_scale(src, dst, ss, width):
            junk_ = junk
            kb.op('act', lambda e: e.activation(out=junk_[:, 0:width], in_=src[:, 0:width], func=AF.Square, accum_out=ss[:]), reads=[src], writes=[junk_, ss])
            kb.op('act', lambda e: e.activation(out=ss[:], in_=ss[:], func=AF.Sqrt, scale=1.0 / width, bias=1e-6), reads=[ss], writes=[ss])
            kb.op('dve', lambda e: e.reciprocal(out=ss[:], in_=ss[:]), reads=[ss], writes=[ss])
            kb.op('dve', lambda e: e.tensor_scalar(out=dst[:, 0:width], in0=src[:, 0:width], scalar1=ss[:, 0:1], scalar2=None, op0=ALU.mult),
                  reads=[src, ss], writes=[dst])

        junk = kb.sb([128, DFF], F32, 'junk')
        ss = kb.sb([128, 1], F32, 'ss')
        hT = kb.sb([128, 22, 128], BF16, 'hT')
        prep_w('kv', wkv, 8, 2 * D, gmem_sb)
        for b_ in range(3):
            prep_w(f'br{b_}', wbr[b_], 8, D)
        prep_w('out', wout, 8, D)
        prep_w('fi', wfi, 8, 2 * DFF, gffn_sb)
        prep_w('fo', wfo, 22, D)
        kb.finish(wb)
        mkT = kb.sb([128, 8, 256], BF16, 'mkT')
        mv = kb.sb([128, 2, 1024], BF16, 'mv')
        mt = kb.sb([128, D], F32, 'mt')
        mn = kb.sb([128, D], F32, 'mn')
        mkrow = kb.sb([128, D], F32, 'mkrow')
        for mc in range(2):
            kb.dma(mt[:], mem[mc * 128:(mc + 1) * 128, :], mt, True)
            rms_scale(mt, mn, ss, D)
            transp(mn, 8, hT)
            for c0 in range(0, 2 * D, 256):
                pm = lin(hT, 8, 'kv', c0, 256)
                if c0 < D:
                    kb.op('dve', lambda e: e.tensor_copy(out=mkrow[:, c0:c0 + 256], in_=pm[:, 0:256]), reads=[pm], writes=[mkrow])
                else:
                    kb.op('dve', lambda e: e.tensor_copy(out=mv[:, mc, c0 - D:c0 - D + 256], in_=pm[:, 0:256]), reads=[pm], writes=[mv])
            for c0 in range(0, 8, 4):
                pt = banks[cnt['tp'] % 2]
                cnt['tp'] += 1
                for j in range(4):
                    kb.op('pe', lambda e: e.transpose(out=pt[:, j * 128:(j + 1) * 128], in_=mkrow[:, (c0 + j) * 128:(c0 + j + 1) * 128], identity=ident[:]),
                          reads=[mkrow, ident], writes=[pt])
                kb.op('act', lambda e: e.activation(out=mkT[:, c0:c0 + 4, mc * 128:(mc + 1) * 128], in_=pt[:].rearrange("p (a b) -> p a b", a=4), func=AF.Copy),
                      reads=[pt], writes=[mkT])

        def tile_in(name, w_):
            return kb.sb([128, w_], F32, name)
        xt = tile_in('xt', D); xqt = tile_in('xqt', D); glt = tile_in('glt', 3 * D); odt = tile_in('odt', D); ost = tile_in('ost', D)
        oxa = tile_in('oxa', D); merged = tile_in('merged', D); tmpm = tile_in('tmpm', 256); x1 = tile_in('x1', D); xn = tile_in('xn', D)
        act = tile_in('act', DFF); sil = tile_in('sil', 256); x2 = [tile_in(f'x2_{i}', D) for i in range(2)]
        PTm = kb.sb([128, 8, 128], BF16, 'PTm')
        rsum = kb.sb([128, 4], F32, 'rsum')
        for i in range(nt):
            r = slice(i * 128, (i + 1) * 128)
            for t_, src in ((xt, x), (xqt, xq), (glt, gl), (odt, odn), (ost, osa)):
                kb.dma(t_[:], src[r, :], t_, True)
            transp(xqt, 8, hT)
            for hp in range(2):
                pl = banks[6 + hp]
                for hh in range(2):
                    h = hp * 2 + hh
                    for mc in range(2):
                        col = (hh * 2 + mc) * 128
                        for dc in range(2):
                            kb.op('pe', lambda e: e.matmul(pl[:, col:col + 128], lhsT=mkT[:, h * 2 + dc, mc * 128:(mc + 1) * 128], rhs=hT[:, h * 2 + dc, :],
                                                           start=(dc == 0), stop=(dc == 1)), reads=[mkT, hT], writes=[pl])
                kb.op('act', lambda e: e.activation(out=PTm[:, hp * 4:(hp + 1) * 4, :], in_=pl[:].rearrange("p (a b) -> p a b", a=4), func=AF.Exp, scale=256.0 ** -0.5),
                      reads=[pl], writes=[PTm])
            pv = [banks[2], banks[3]]
            psm = banks[4]
            for h in range(4):
                for mc in range(2):
                    kb.op('pe', lambda e: e.matmul(pv[h // 2][:, (h % 2) * 256:(h % 2) * 256 + 256], lhsT=PTm[:, h * 2 + mc, :], rhs=mv[:, mc, h * 256:(h + 1) * 256],
                                                   start=(mc == 0), stop=(mc == 1)), reads=[PTm, mv], writes=[pv[h // 2]])
                for mc in range(2):
                    kb.op('pe', lambda e: e.matmul(psm[:, h:h + 1], lhsT=PTm[:, h * 2 + mc, :], rhs=ones_b[:], start=(mc == 0), stop=(mc == 1)),
                          reads=[PTm, ones_b], writes=[psm])
            kb.op('dve', lambda e: e.reciprocal(out=rsum[:], in_=psm[:, 0:4]), reads=[psm], writes=[rsum])
            for h in range(4):
                kb.op('dve', lambda e: e.tensor_scalar(out=oxa[:, h * 256:(h + 1) * 256], in0=pv[h // 2][:, (h % 2) * 256:(h % 2) * 256 + 256],
                                                       scalar1=rsum[:, h:h + 1], scalar2=None, op0=ALU.mult), reads=[pv[h // 2], rsum], writes=[oxa])
            kb.op('dve', lambda e: e.tensor_tensor(out=glt[:], in0=glt[:], in1=bg_sb[:], op=ALU.add), reads=[glt, bg_sb], writes=[glt])
            kb.op('act', lambda e: e.activation(out=glt[:], in_=glt[:], func=AF.Sigmoid), reads=[glt], writes=[glt])
            for b, src in enumerate((odt, ost, oxa)):
                transp(src, 8, hT)
                for c0 in range(0, D, 256):
                    pm = lin(hT, 8, f'br{b}', c0, 256)
                    if b == 0:
                        kb.op('dve', lambda e: e.tensor_tensor(out=merged[:, c0:c0 + 256], in0=pm[:, 0:256], in1=glt[:, b * D + c0:b * D + c0 + 256], op=ALU.mult),
                              reads=[pm, glt], writes=[merged])
                    else:
                        kb.op('dve', lambda e: e.tensor_tensor(out=tmpm[:], in0=pm[:, 0:256], in1=glt[:, b * D + c0:b * D + c0 + 256], op=ALU.mult),
                              reads=[pm, glt], writes=[tmpm])
                        kb.op('pool', lambda e: e.tensor_tensor(out=merged[:, c0:c0 + 256], in0=merged[:, c0:c0 + 256], in1=tmpm[:], op=ALU.add),
                              reads=[merged, tmpm], writes=[merged])
            transp(merged, 8, hT)
            for c0 in range(0, D, 256):
                pm = lin(hT, 8, 'out', c0, 256)
                kb.op('dve', lambda e: e.tensor_tensor(out=x1[:, c0:c0 + 256], in0=pm[:, 0:256], in1=xt[:, c0:c0 + 256], op=ALU.add), reads=[pm, xt], writes=[x1])
            rms_scale(x1, xn, ss, D)
            transp(xn, 8, hT)
            for c0 in range(0, DFF, 256):
                pg_ = lin(hT, 8, 'fi', c0, 256)
                kb.op('act', lambda e: e.activation(out=sil[:], in_=pg_[:, 0:256], func=AF.Silu), reads=[pg_], writes=[sil])
                pu_ = lin(hT, 8, 'fi', DFF + c0, 256)
                kb.op('dve', lambda e: e.tensor_tensor(out=act[:, c0:c0 + 256], in0=pu_[:, 0:256], in1=sil[:], op=ALU.mult), reads=[pu_, sil], writes=[act])
            transp(act, 22, hT)
            xo = x2[i % 2]
            for c0 in range(0, D, 256):
                pm = lin(hT, 22, 'fo', c0, 256)
                kb.op('dve', lambda e: e.tensor_tensor(out=xo[:, c0:c0 + 256], in0=pm[:, 0:256], in1=x1[:, c0:c0 + 256], op=ALU.add), reads=[pm, x1], writes=[xo])
            if final:
                rms_scale(xo, xn, ss, D)
                kb.op('dve', lambda e: e.tensor_tensor(out=xo[:], in0=xn[:], in1=gfin_sb[:], op=ALU.mult), reads=[xn, gfin_sb], writes=[xo])
            kb.dma(y[r, :], xo[:], xo, False)
        kb.finish(x2)
    return nc


def _col8(v):
    return np.ascontiguousarray(np.asarray(v, np.float32).reshape(8, 128).T)


def kernel(x, mem, positions, norm_mix, norm_mem, w_in, b_gate, conv_w, a_log, dt_bias,
           dn_norm, w_mem_kv, w_branch, w_out, norm_ffn, w_ffn_in, w_ffn_out, norm_final):
    f32 = np.float32
    x = np.asarray(x, f32)
    mem = np.asarray(mem, f32)
    positions = np.asarray(positions, np.int32)
    ncores = 8
    cores = list(range(ncores))
    S = 16384
    NT = 4096
    isa, iix = host_consts()
    ncA = build_A(NT)
    ncB = build_B(NH=2)
    ncC = build_C(32)
    xcur = x
    for l in range(2):
        maps = []
        for c in cores:
            b, j = c // 4, c % 4
            r = slice(j * NT, (j + 1) * NT)
            maps.append(dict(x=np.ascontiguousarray(xcur[b, r]),
                             pos=np.ascontiguousarray(positions[b, r].reshape(NT // 128, 128).T),
                             gcol=_col8(norm_mix[l]), w=np.asarray(w_in[l], f32), invf_sa=isa, invf_ix=iix))
        res = run_bass_kernel_spmd(ncA, maps, core_ids=cores)
        P = np.stack([np.concatenate([res.results[b * 4 + j]["P"] for j in range(4)], 0) for b in range(2)])
        Pb = np.stack([np.concatenate([res.results[b * 4 + j]["Pb"] for j in range(4)], 0) for b in range(2)])
        del res
        cw_l = np.asarray(conv_w[l], f32)
        maps = []
        for c in cores:
            b, hp_ = c // 4, c % 4
            xp = np.zeros((2, S + 3, 384), f32)
            z = np.empty((2, S, 128), f32)
            ba = np.empty((2, 128, 2, S // 128), f32)
            wc = np.empty((2, 128, 4, 384), f32)
            hp = np.empty((2, 128, 2), f32)
            for i in range(2):
                h = hp_ * 2 + i
                for t in range(3):
                    xp[i, 3:, t * 128:(t + 1) * 128] = P[b, :, t * 1024 + h * 128:t * 1024 + (h + 1) * 128]
                    wc[i, :, :, t * 128:(t + 1) * 128] = cw_l[None, :, t * 1024 + h * 128:t * 1024 + (h + 1) * 128]
                z[i] = P[b, :, 3072 + h * 128:3072 + (h + 1) * 128]
                ba[i, :, 0, :] = P[b, :, 4096 + h].reshape(S // 128, 128).T
                ba[i, :, 1, :] = P[b, :, 4104 + h].reshape(S // 128, 128).T
                hp[i, :, 0] = a_log[l][h]
                hp[i, :, 1] = dt_bias[l][h]
            maps.append(dict(xp=xp, z=z, ba=ba, wc=wc, hp=hp,
                             dnw=np.ascontiguousarray(np.tile(np.asarray(dn_norm[l], f32)[None], (128, 1)))))
        res = run_bass_kernel_spmd(ncB, maps, core_ids=cores)
        odn = np.empty((2, S, 1024), f32)
        for c in cores:
            b, hp_ = c // 4, c % 4
            for i in range(2):
                h = hp_ * 2 + i
                odn[b, :, h * 128:(h + 1) * 128] = res.results[c]["o"][i]
        del res, maps
        maps = []
        toks = []
        for b in range(2):
            kT = np.ascontiguousarray(Pb[b][:, 1024:2048].reshape(S, 8, 128).transpose(1, 2, 0))
            vv = np.ascontiguousarray(Pb[b][:, 2048:3072].reshape(128, 128, 8, 128).transpose(2, 1, 0, 3))
            ikT = np.ascontiguousarray(P[b][:, 7696:7760].T)
            for j in range(4):
                tk = np.concatenate([np.arange((4 * k + j) * 128, (4 * k + j + 1) * 128) for k in range(32)])
                toks.append(tk)
                maps.append(dict(iqT=np.ascontiguousarray(P[b][tk, 7184:7696].reshape(-1, 8, 64).transpose(2, 1, 0)),
                                 iw=np.ascontiguousarray(P[b][tk, 7760:7768].reshape(32, 128, 8).transpose(1, 0, 2)),
                                 ikT=ikT,
                                 qT=np.ascontiguousarray(Pb[b][tk, 0:1024].reshape(-1, 8, 128).transpose(2, 1, 0)),
                                 kT=kT, v=vv, admis=admis_mask(j)))
        res = run_bass_kernel_spmd(ncC, maps, core_ids=cores)
        osa = np.empty((2, S, 1024), f32)
        for c in cores:
            osa[c // 4][toks[c]] = res.results[c]["o"]
        del res, maps
        ncD = build_D(NT, final=(l == 1))
        maps = []
        for c in cores:
            b, j = c // 4, c % 4
            r = slice(j * NT, (j + 1) * NT)
            maps.append(dict(x=np.ascontiguousarray(xcur[b, r]), xq=np.ascontiguousarray(P[b, r, 7768:8792]),
                             gl=np.ascontiguousarray(P[b, r, 8792:11864]), odn=np.ascontiguousarray(odn[b, r]),
                             osa=np.ascontiguousarray(osa[b, r]), mem=np.ascontiguousarray(mem[b]), gmem=_col8(norm_mem[l]),
                             wkv=np.asarray(w_mem_kv[l], f32), wbr=np.asarray(w_branch[l], f32), wout=np.asarray(w_out[l], f32),
                             bg=np.ascontiguousarray(np.tile(np.asarray(b_gate[l], f32).reshape(1, -1), (128, 1))),
                             gffn=_col8(norm_ffn[l]), wfi=np.asarray(w_ffn_in[l], f32), wfo=np.asarray(w_ffn_out[l], f32),
                             gfin=np.ascontiguousarray(np.tile(np.asarray(norm_final, f32)[None], (128, 1)))))
        res = run_bass_kernel_spmd(ncD, maps, core_ids=cores)
        xcur = np.stack([np.concatenate([res.results[b * 4 + j]["y"] for j in range(4)], 0) for b in range(2)])
        del res, maps, P, Pb, odn, osa
    return xcur.astype(np.float32)
```
